# Optimizing a Trainium2 kernel written in Bass

```python
import math
import jax, jax.numpy as jnp
from jax import lax
import numpy as np

D_MODEL = 1024
BATCH = 4
SEQ = 4096
DEPTH = 1

CHUNK = 64
Q_BLOCK = 128
EPS = 1e-6
MLA_HEADS = 8
MLA_NOPE = 64
MLA_ROPE = 32
MLA_QK = MLA_NOPE + MLA_ROPE
MLA_V = 64
MLA_Q_LORA = 384
MLA_KV_LORA = 256
ROPE_THETA = 10000.0
DIFF_HEADS = 4
DIFF_HEAD_DIM = 64
DIFF_V = 2 * DIFF_HEAD_DIM
REL_BUCKETS = 32
REL_MAX_DIST = 128
MIX_WIDTH = MLA_HEADS * MLA_V + DIFF_HEADS * DIFF_V
IN_WIDTHS = (MLA_Q_LORA, MLA_KV_LORA, MLA_ROPE,
             DIFF_HEADS * 2 * DIFF_HEAD_DIM, DIFF_HEADS * 2 * DIFF_HEAD_DIM, DIFF_HEADS * DIFF_V)
IN_WIDTH = sum(IN_WIDTHS)
IN_SPLITS = np.cumsum(IN_WIDTHS)[:-1].tolist()
N_GROUPS = 4
EXPERTS_PER_GROUP = 4
N_EXPERTS = N_GROUPS * EXPERTS_PER_GROUP
TOP_K = 2
D_EXPERT = 512

kernel_name = "hymba_mla_diffattn_hmoe_adaln"


def _rms(x, g):
    xf = x.astype(jnp.float32)
    y = xf * lax.rsqrt(jnp.mean(xf * xf, axis=-1, keepdims=True) + EPS)
    return (y * g.astype(jnp.float32)).astype(x.dtype)


def _to_blocks(a):
    b, s = a.shape[0], a.shape[1]
    return jnp.moveaxis(a.reshape(b, s // Q_BLOCK, Q_BLOCK, *a.shape[2:]), 1, 0)


def _from_blocks(a):
    a = jnp.moveaxis(a, 0, 1)
    return a.reshape(a.shape[0], a.shape[1] * a.shape[2], *a.shape[3:])


def _rope(x, cos, sin):
    x1, x2 = jnp.split(x, 2, axis=-1)
    return jnp.concatenate([x1 * cos - x2 * sin, x2 * cos + x1 * sin], axis=-1)


def _rel_bucket(rel):
    nb = REL_BUCKETS // 2
    max_exact = nb // 2
    n = jnp.abs(rel)
    large = max_exact + (jnp.log(jnp.maximum(n, 1).astype(jnp.float32) / max_exact)
                         / math.log(REL_MAX_DIST / max_exact) * (nb - max_exact)).astype(jnp.int32)
    large = jnp.minimum(large, nb - 1)
    return jnp.where(rel > 0, nb, 0) + jnp.where(n < max_exact, n, large)


def _mla(c_q, c_kv, k_pe, positions, cq_g, w_uq, ckv_g, w_ukv, q_g, k_g):
    B, S, _ = c_q.shape
    q = (_rms(c_q, cq_g) @ w_uq).reshape(B, S, MLA_HEADS, MLA_QK)
    kv = (_rms(c_kv, ckv_g) @ w_ukv).reshape(B, S, MLA_HEADS, MLA_NOPE + MLA_V)
    k_nope, v = kv[..., :MLA_NOPE], kv[..., MLA_NOPE:]
    k = jnp.concatenate([k_nope, jnp.broadcast_to(k_pe[:, :, None, :], (B, S, MLA_HEADS, MLA_ROPE))], axis=-1)
    q, k = _rms(q, q_g), _rms(k, k_g)
    inv_freq = ROPE_THETA ** (-jnp.arange(0, MLA_ROPE // 2, dtype=jnp.float32) / (MLA_ROPE // 2))
    ang = positions.astype(jnp.float32)[..., None] * inv_freq
    cos = jnp.cos(ang)[:, :, None, :].astype(q.dtype)
    sin = jnp.sin(ang)[:, :, None, :].astype(q.dtype)
    q = jnp.concatenate([q[..., :MLA_NOPE], _rope(q[..., MLA_NOPE:], cos, sin)], axis=-1)
    k = jnp.concatenate([k[..., :MLA_NOPE], _rope(k[..., MLA_NOPE:], cos, sin)], axis=-1)
    scale = MLA_QK ** -0.5
    key_chunk = jnp.arange(S) // CHUNK
    q_chunks = key_chunk.reshape(S // Q_BLOCK, Q_BLOCK)

    def block(args):
        qb, qc = args
        s = jnp.einsum('bqhd,bkhd->bhqk', qb, k).astype(jnp.float32) * scale
        s = jnp.where(key_chunk[None, :] <= qc[:, None], s, -jnp.inf)
        p = jax.nn.softmax(s, axis=-1).astype(v.dtype)
        return jnp.einsum('bhqk,bkhe->bqhe', p, v)

    o = _from_blocks(lax.map(block, (_to_blocks(q), q_chunks)))
    return o.reshape(B, S, MLA_HEADS * MLA_V)


def _diff_attn(dq, dk, dv, positions, rel_bias, q_g, k_g, lq1, lk1, lq2, lk2, subln_g, lambda_init):
    B, S, _ = dq.shape
    q = _rms(dq.reshape(B, S, DIFF_HEADS, 2, DIFF_HEAD_DIM), q_g)
    k = _rms(dk.reshape(B, S, DIFF_HEADS, 2, DIFF_HEAD_DIM), k_g)
    v = dv.reshape(B, S, DIFF_HEADS, DIFF_V)
    lam = (jnp.exp(jnp.sum(lq1.astype(jnp.float32) * lk1.astype(jnp.float32)))
           - jnp.exp(jnp.sum(lq2.astype(jnp.float32) * lk2.astype(jnp.float32))) + lambda_init)
    scale = DIFF_HEAD_DIM ** -0.5
    key_chunk = jnp.arange(S) // CHUNK
    q_chunks = key_chunk.reshape(S // Q_BLOCK, Q_BLOCK)

    def block(args):
        qb, qpos, qc = args
        s = jnp.einsum('bqhmd,bkhmd->bhmqk', qb, k).astype(jnp.float32) * scale
        bucket = _rel_bucket(positions[:, None, :] - qpos[:, :, None])
        bias = jnp.moveaxis(rel_bias[bucket], -1, 1).astype(jnp.float32)
        s = jnp.where(key_chunk[None, :] <= qc[:, None], s + bias[:, :, None], -jnp.inf)
        p = jax.nn.softmax(s, axis=-1)
        a = (p[:, :, 0] - lam * p[:, :, 1]).astype(v.dtype)
        return jnp.einsum('bhqk,bkhe->bqhe', a, v)

    o = _from_blocks(lax.map(block, (_to_blocks(q), _to_blocks(positions), q_chunks)))
    o = _rms(o, subln_g) * (1.0 - lambda_init)
    return o.reshape(B, S, DIFF_HEADS * DIFF_V)


def _hmoe(h, w_rg, b_rg, w_re, b_re, w_gate, w_up, w_down):
    B, S, D = h.shape
    t = h.reshape(B * S, D)
    g_logits = (t @ w_rg + b_rg).astype(jnp.float32)
    g_idx = jnp.argmax(g_logits, axis=-1)
    g_w = jnp.take_along_axis(jax.nn.softmax(g_logits, axis=-1), g_idx[:, None], axis=-1)
    e_logits = (t @ w_re + b_re).astype(jnp.float32).reshape(-1, N_GROUPS, EXPERTS_PER_GROUP)
    e_sel = jnp.take_along_axis(e_logits, g_idx[:, None, None], axis=1)[:, 0]
    top_v, top_i = lax.top_k(e_sel, TOP_K)
    top_w = jax.nn.softmax(top_v, axis=-1) * g_w
    gate_in = jnp.sum(jax.nn.one_hot(top_i, EXPERTS_PER_GROUP, dtype=jnp.float32) * top_w[..., None], axis=1)
    gate = (jax.nn.one_hot(g_idx, N_GROUPS, dtype=jnp.float32)[:, :, None]
            * gate_in[:, None, :]).reshape(-1, N_EXPERTS).astype(h.dtype)
    y = jnp.zeros_like(t)
    for e in range(N_EXPERTS):
        he = jax.nn.silu(t @ w_gate[e]) * (t @ w_up[e])
        y = y + gate[:, e:e + 1] * (he @ w_down[e])
    return y.reshape(B, S, D)


def setup_inputs(seed: int = 0) -> dict:
    key = jax.random.key(seed)
    ks = jax.random.split(key, 40)
    D, L, f32 = D_MODEL, DEPTH, jnp.float32

    def nrm(k, shape, fan_in, mult=1.0):
        return jax.random.normal(k, shape, f32) * (mult * fan_in ** -0.5)

    def gain(k, shape):
        return 1.0 + 0.05 * jax.random.normal(k, shape, f32)

    def small(k, shape, s):
        return s * jax.random.normal(k, shape, f32)

    offs = jax.random.randint(ks[2], (BATCH, 1), 0, 1024, dtype=jnp.int32)
    return {
        "x": jax.random.normal(ks[0], (BATCH, SEQ, D), f32),
        "c": jax.random.normal(ks[1], (BATCH, D), f32),
        "positions": (offs + jnp.arange(SEQ, dtype=jnp.int32)[None, :]).astype(jnp.int32),
        "rel_bias": small(ks[3], (REL_BUCKETS, DIFF_HEADS), 0.5),
        "w_ada": nrm(ks[4], (L, D, 6 * D), D, 0.5),
        "b_ada": small(ks[5], (L, 6 * D), 0.02),
        "norm1_g": gain(ks[6], (L, D)),
        "w_in": nrm(ks[7], (L, D, IN_WIDTH), D),
        "mla_cq_g": gain(ks[8], (L, MLA_Q_LORA)),
        "w_uq": nrm(ks[9], (L, MLA_Q_LORA, MLA_HEADS * MLA_QK), MLA_Q_LORA),
        "mla_ckv_g": gain(ks[10], (L, MLA_KV_LORA)),
        "w_ukv": nrm(ks[11], (L, MLA_KV_LORA, MLA_HEADS * (MLA_NOPE + MLA_V)), MLA_KV_LORA),
        "mla_q_g": gain(ks[12], (L, MLA_QK)),
        "mla_k_g": gain(ks[13], (L, MLA_QK)),
        "diff_q_g": gain(ks[14], (L, DIFF_HEAD_DIM)),
        "diff_k_g": gain(ks[15], (L, DIFF_HEAD_DIM)),
        "lambda_q1": small(ks[16], (L, DIFF_HEAD_DIM), 0.1),
        "lambda_k1": small(ks[17], (L, DIFF_HEAD_DIM), 0.1),
        "lambda_q2": small(ks[18], (L, DIFF_HEAD_DIM), 0.1),
        "lambda_k2": small(ks[19], (L, DIFF_HEAD_DIM), 0.1),
        "diff_subln_g": gain(ks[20], (L, DIFF_V)),
        "w_o": nrm(ks[21], (L, MIX_WIDTH, D), MIX_WIDTH),
        "norm2_g": gain(ks[22], (L, D)),
        "w_rg": nrm(ks[23], (L, D, N_GROUPS), D),
        "b_rg": small(ks[24], (L, N_GROUPS), 0.01),
        "w_re": nrm(ks[25], (L, D, N_EXPERTS), D),
        "b_re": small(ks[26], (L, N_EXPERTS), 0.01),
        "w_gate": nrm(ks[27], (L, N_EXPERTS, D, D_EXPERT), D),
        "w_up": nrm(ks[28], (L, N_EXPERTS, D, D_EXPERT), D),
        "w_down": nrm(ks[29], (L, N_EXPERTS, D_EXPERT, D), D_EXPERT),
    }


def reference(x, c, positions, rel_bias, w_ada, b_ada, norm1_g, w_in, mla_cq_g, w_uq, mla_ckv_g, w_ukv,
              mla_q_g, mla_k_g, diff_q_g, diff_k_g, lambda_q1, lambda_k1, lambda_q2, lambda_k2,
              diff_subln_g, w_o, norm2_g, w_rg, b_rg, w_re, b_re, w_gate, w_up, w_down):
    for l in range(DEPTH):
        lambda_init = 0.8 - 0.6 * math.exp(-0.3 * l)
        mod = jnp.einsum('bd,de->be', jax.nn.silu(c), w_ada[l]) + b_ada[l]
        sh1, sc1, g1, sh2, sc2, g2 = [m[:, None, :] for m in jnp.split(mod, 6, axis=-1)]
        h = _rms(x, norm1_g[l]) * (1.0 + sc1) + sh1
        proj = h @ w_in[l]
        c_q, c_kv, k_pe, dq, dk, dv = jnp.split(proj, IN_SPLITS, axis=-1)
        o_mla = _mla(c_q, c_kv, k_pe, positions, mla_cq_g[l], w_uq[l], mla_ckv_g[l], w_ukv[l],
                     mla_q_g[l], mla_k_g[l])
        o_diff = _diff_attn(dq, dk, dv, positions, rel_bias, diff_q_g[l], diff_k_g[l], lambda_q1[l],
                            lambda_k1[l], lambda_q2[l], lambda_k2[l], diff_subln_g[l], lambda_init)
        x = x + g1 * (jnp.concatenate([o_mla, o_diff], axis=-1) @ w_o[l])
        h2 = _rms(x, norm2_g[l]) * (1.0 + sc2) + sh2
        x = x + g2 * _hmoe(h2, w_rg[l], b_rg[l], w_re[l], b_re[l], w_gate[l], w_up[l], w_down[l])
    return x
```

```python
import math
from contextlib import ExitStack

import numpy as np
import concourse.bass as bass
import concourse.mybir as mybir
from concourse.bass_utils import run_bass_kernel_spmd

F32 = mybir.dt.float32
BF16 = mybir.dt.bfloat16
I32 = mybir.dt.int32
AF = mybir.ActivationFunctionType
ALU = mybir.AluOpType
AX = mybir.AxisListType

EPS = 1e-6
NEG = -30000.0
LAMBDA_INIT = 0.8 - 0.6 * math.exp(-0.3 * 0)
N_EXP_RUN = 16

ENGS = ("pe", "act", "dve", "pool", "sp")


class Op:
    __slots__ = ("eng", "fn", "deps", "idx", "signal", "count", "dma_key", "dma_count", "is_dma")

    def __init__(self, eng, fn, deps):
        self.eng, self.fn, self.deps = eng, fn, deps
        self.idx, self.signal, self.count = -1, False, 0
        self.is_dma, self.dma_key, self.dma_count = False, None, 0


class Prog:
    def __init__(self):
        self.ops = {e: [] for e in ENGS}
        self.dma_counts = {}
        self.last_w = {}
        self.readers = {}

    def op(self, eng, fn, reads=(), writes=(), dma_key=None):
        ps_reads = [k for k in reads if isinstance(k, str) and k.startswith("ps") and k[2:].isdigit()]
        if ps_reads:
            reads = [k for k in reads if k not in ps_reads]
            writes = list(writes) + [k for k in ps_reads if k not in writes]
        deps = []
        for k in reads:
            w = self.last_w.get(k)
            if w is not None:
                deps.append(w)
        for k in writes:
            w = self.last_w.get(k)
            if w is not None:
                deps.append(w)
            deps.extend(self.readers.get(k, ()))
        o = Op(eng, fn, list(dict.fromkeys(deps)))
        o.idx = len(self.ops[eng])
        self.ops[eng].append(o)
        if dma_key is not None:
            o.is_dma = True
            o.dma_key = dma_key
            self.dma_counts[dma_key] = self.dma_counts.get(dma_key, 0) + 16
            o.dma_count = self.dma_counts[dma_key]
        for k in reads:
            self.readers.setdefault(k, []).append(o)
        for k in writes:
            self.last_w[k] = o
            self.readers[k] = []
        return o

    def emit(self, nc, stack):
        for e in ENGS:
            for o in self.ops[e]:
                latest = {}
                for d in o.deps:
                    if d.is_dma:
                        continue
                    if d.eng == o.eng and o.eng == "pe":
                        continue
                    if d.eng not in latest or d.idx > latest[d.eng].idx:
                        latest[d.eng] = d
                o.deps = [d for d in o.deps if d.is_dma] + list(latest.values())
                for d in latest.values():
                    d.signal = True
        sems = {}
        for e in ENGS:
            sems[e] = stack.enter_context(nc.semaphore("s_" + e))
            c = 0
            for o in self.ops[e]:
                if o.signal:
                    c += 1
                    o.count = c
        for k in self.dma_counts:
            sems["dma_" + k] = stack.enter_context(nc.semaphore("d_" + k))
        block = stack.enter_context(nc.Block())
        engmap = {"pe": "tensor", "act": "scalar", "dve": "vector", "pool": "gpsimd", "sp": "sync"}

        def make(e):
            def body(eng):
                waited = {}
                for o in self.ops[e]:
                    need = {}
                    for d in o.deps:
                        if d.is_dma:
                            sk, val = "dma_" + d.dma_key, d.dma_count
                        else:
                            if d.eng == e and e == "pe":
                                continue
                            sk, val = d.eng, d.count
                        need[sk] = max(need.get(sk, 0), val)
                    for sk, val in need.items():
                        if waited.get(sk, 0) >= val:
                            continue
                        eng.wait_ge(sems[sk], val)
                        waited[sk] = val
                    ins = o.fn(eng)
                    if o.is_dma:
                        ins.then_inc(sems["dma_" + o.dma_key], 16)
                    elif o.signal:
                        ins.then_inc(sems[e], 1)
            return body

        self.stats = {e: (len(self.ops[e]), max([o.count for o in self.ops[e]] + [0])) for e in ENGS}
        for e in ENGS:
            if self.ops[e]:
                getattr(block, engmap[e])(make(e))


def build_program(stop=None):
    nc = bass.Bass("TRN2", target_bir_lowering=False)

    def din(name, shape, dt=F32):
        return nc.dram_tensor(name, shape, dt, kind="ExternalInput").ap()

    xp = din("xp", [4096, 1024])
    posp = din("posp", [1, 4096], I32)
    vrows = din("vrows", [96, 128])
    cst = din("cst", [128, 8])
    mb = din("mb", [128, 48])
    rel_bias = din("rel_bias", [1, 128])
    w_ada = din("w_ada", [1024, 6144])
    w_in = din("w_in", [1024, 2208])
    w_uq = din("w_uq", [384, 768])
    w_ukv = din("w_ukv", [256, 1024])
    w_o = din("w_o", [1024, 1024])
    w_r = din("w_r", [1024, 20])
    b_r = din("b_r", [1, 20])
    w_gate = din("w_gate", [16, 1024, 512])
    w_up = din("w_up", [16, 1024, 512])
    w_down = din("w_down", [16, 512, 1024])
    out = nc.dram_tensor("out", [2048, 1024], F32, kind="ExternalOutput").ap()

    st = ExitStack()
    with st:
        KB = 512
        arena = st.enter_context(nc.sbuf_tensor("arena", [128, 200 * KB], BF16))

        def region(off_kb, size_kb):
            return arena[:, int(off_kb * KB):int((off_kb + size_kb) * KB)]

        U1 = region(0, 64)
        U2 = region(64, 48)
        AT = region(112, 32)
        RA = region(144, 16)
        HB = region(160, 20)
        BC = region(180, 12)
        MI = region(192, 8)

        def sb(name, shape, dt=F32):
            return st.enter_context(nc.sbuf_tensor(name, shape, dt))

        ident_f = sb("ident_f", [128, 128])
        ident_b = sb("ident_b", [128, 128], BF16)
        ones_f = sb("ones_f", [128, 128])
        ones_b = sb("ones_b", [128, 128], BF16)
        blk64 = sb("blk64", [128, 128], BF16)
        V = sb("V", [96, 128])
        col = sb("col", [128, 96])
        cstt = sb("cstt", [128, 8])
        mbt = sb("mbt", [128, 48])
        sil_b = sb("sil_b", [128, 8], BF16)
        modcol = sb("modcol", [128, 48])
        gcol = sb("gcol", [128, 16])
        misc = sb("misc", [128, 16])
        stat = sb("stat", [128, 64])
        stat2 = sb("stat2", [128, 64])
        gate = sb("gate", [128, 16 * 16])
        rt = sb("rt", [128, 64])
        wr_f = sb("wr_f", [128, 8 * 20])
        br_f = sb("br_f", [1, 20])
        relb = sb("relb", [128, 128])
        dgt = sb("dgt", [128, 2 * 128])
        steps = U2[:, 19200:19712].bitcast(F32)
        kpeW = U2[:, 17664:18432]
        kpeWr = U2[:, 18432:19200]

        ps = [st.enter_context(nc.psum_tensor("ps%d" % i, [128, 512], F32)) for i in range(8)]

        p = Prog()

        def finish():
            x1f = U1.bitcast(F32).rearrange("p (s n) -> p s n", s=16)
            outs_ = []
            for s_ in range(16):
                outs_.append(p.op("sp", (lambda s__: (lambda e: e.dma_start(out=out[s__ * 128:(s__ + 1) * 128, :],
                                                                           in_=x1f[:, s__, :])))(s_),
                                  [("x1", s_)] + [("hT", b_) for b_ in range(8)], [], dma_key="out"))
            fin_ = p.op("sp", lambda e: e.nop(), [], [])
            fin_.deps.extend(outs_)
            p.emit(nc, st)
            nc._prog_stats = (p.stats, dict(p.dma_counts))
            return nc

        def mm(out_ap, lhsT, rhs, start, stop, reads, writes):
            return p.op("pe", lambda e: e.matmul(out_ap, lhsT=lhsT, rhs=rhs, start=start, stop=stop,
                                                 skip_group_check=True), reads, writes)

        def tr(out_ap, in_ap, ident, reads, writes):
            return p.op("pe", lambda e: e.transpose(out_ap, in_ap, ident), reads, writes)

        def act(out_ap, in_ap, func, reads, writes, **kw):
            return p.op("act", lambda e: e.activation(out=out_ap, in_=in_ap, func=func, **kw), reads, writes)

        def tt(eng, out_ap, a, b, op, reads, writes):
            return p.op(eng, lambda e: e.tensor_tensor(out=out_ap, in0=a, in1=b, op=op), reads, writes)

        def ts(eng, out_ap, a, s1, s2, op0, op1, reads, writes):
            if op1 is None:
                return p.op(eng, lambda e: e.tensor_scalar(out=out_ap, in0=a, scalar1=s1, scalar2=None, op0=op0),
                            reads, writes)
            return p.op(eng, lambda e: e.tensor_scalar(out=out_ap, in0=a, scalar1=s1, scalar2=s2, op0=op0, op1=op1),
                        reads, writes)

        def stt(out_ap, a, s, b, op0, op1, reads, writes):
            return p.op("dve", lambda e: e.scalar_tensor_tensor(out=out_ap, in0=a, scalar=s, in1=b, op0=op0, op1=op1),
                        reads, writes)

        def cp(eng, out_ap, in_ap, reads, writes):
            if eng == "act":
                return p.op("act", lambda e: e.copy(out=out_ap, in_=in_ap), reads, writes)
            return p.op(eng, lambda e: e.tensor_copy(out=out_ap, in_=in_ap), reads, writes)

        def memset(eng, ap, val, reads, writes):
            return p.op(eng, lambda e: e.memset(ap, val), reads, writes)

        def dma(q, out_ap, in_ap, key, reads, writes):
            return p.op(q, lambda e: e.dma_start(out=out_ap, in_=in_ap), reads, writes, dma_key=key)

        fdummy = sb("fdummy", [128, 8])

        def fence(keys):
            return p.op("dve", lambda e: e.memset(fdummy[:, 0:1], 0.0), [], list(keys) + ["fdummy"])

        def recip(out_ap, in_ap, reads, writes):
            return p.op("dve", lambda e: e.reciprocal(out=out_ap, in_=in_ap), reads, writes)

        def rmax(out_ap, in_ap, reads, writes):
            return p.op("dve", lambda e: e.reduce_max(out=out_ap, in_=in_ap, axis=AX.X), reads, writes)

        epsc = sb("epsc", [128, 1])

        def rsqrt_chain(out_ap, in_ap, inv_n, reads, writes, pr=(0, 128)):
            act(out_ap, in_ap, AF.Ln, list(reads) + ["epsc"], writes, scale=inv_n, bias=epsc[pr[0]:pr[1], :])
            act(out_ap, out_ap, AF.Exp, writes, writes, scale=-0.5)

        memset("pool", ones_f[:], 1.0, [], ["ones_f"])
        memset("pool", epsc[:], EPS, [], ["epsc"])
        p.op("pool", lambda e: e.affine_select(out=ident_f[:], in_=ones_f[:], pattern=[[-1, 128]],
                                               compare_op=ALU.is_equal, fill=0.0, base=0, channel_multiplier=1),
             ["ones_f"], ["ident_f"])
        cp("dve", ident_b[:], ident_f[:], ["ident_f"], ["ident_b"])
        cp("dve", ones_b[:], ones_f[:], ["ones_f"], ["ones_b"])
        memset("dve", blk64[:], 0.0, [], ["blk64"])
        memset("dve", blk64[0:64, 0:64], 1.0, [], ["blk64"])
        memset("dve", blk64[64:128, 64:128], 1.0, [], ["blk64"])
        dma("sp", V[:], vrows[:, :], "V", [], ["V"])
        dma("sp", cstt[:], cst[:, :], "cst", [], ["cst"])
        dma("sp", mbt[:], mb[:, :], "mb", [], ["mb"])
        dma("sp", relb[:], rel_bias[0, :].partition_broadcast(128), "relb", [], ["relb"])
        dma("sp", wr_f[:].rearrange("p (k n) -> p k n", k=8), w_r.rearrange("(k p) n -> p k n", p=128),
            "wr", [], ["wr"])
        dma("sp", br_f[:], b_r[:, :], "br", [], ["br"])
        tr(ps[0][:, 0:96], V[:, :], ident_f[0:96, 0:96], ["V", "ident_f"], ["ps0"])
        cp("act", col[:], ps[0][:, 0:96], ["ps0"], ["col"])
        C_C, C_BADA, C_N1G, C_N2G, C_CQG, C_CKVG = 0, 8, 56, 64, 72, 75
        C_QG, C_KG, C_DQG, C_DKG, C_SUBLN, C_LAM, C_QGR, C_KGR = 77, 78, 79, 80, 81, 82, 86, 87
        act(sil_b[:], col[:, C_C:C_C + 8], AF.Silu, ["col"], ["sil"])

        w_in_v = w_in.rearrange("(k p) n -> p k n", p=128)
        kpeW3 = kpeW.rearrange("p (k n) -> p k n", k=8)
        kpeWr3 = kpeWr.rearrange("p (k n) -> p k n", k=8)
        memset("dve", kpeW, 0.0, [], ["kpeW"])
        memset("dve", kpeWr, 0.0, [], ["kpeWr"])
        dma("pool", kpeW3[:, :, 64:96], w_in_v[:, :, 640:672], "kpeW", [], ["kpeW"])
        dma("pool", kpeWr3[:, :, 64:80], w_in_v[:, :, 656:672], "kpeWr", [], ["kpeWr"])
        dma("pool", kpeWr3[:, :, 80:96], w_in_v[:, :, 640:656], "kpeWr2", [], ["kpeWr"])

        w_ada_v = w_ada.rearrange("(k p) n -> p k n", p=128)
        slab = [U1[:, i * 8192:(i + 1) * 8192].rearrange("p (k n) -> p k n", k=8) for i in range(2)]
        for s in range(6):
            dma("pool", slab[s % 2], w_ada_v[:, :, s * 1024:(s + 1) * 1024], "slab%d" % (s % 2), [],
                [("slab", s % 2)])
            for mloc in range(8):
                m = s * 8 + mloc
                for k in range(8):
                    mm(ps[1][:, m:m + 1], slab[s % 2][:, k, mloc * 128:(mloc + 1) * 128], sil_b[:, k:k + 1],
                       k == 0, k == 7, [("slab", s % 2), "sil"], ["ps1"])
        win = U2[:, 0:8 * 2208].rearrange("p (k n) -> p k n", k=8)
        w_in_v = w_in.rearrange("(k p) n -> p k n", p=128)
        for k in range(8):
            dma("pool", win[:, k, :], w_in_v[:, k, :], "win%d" % k, [], [("win", k)])
        WIN = [("win", k) for k in range(8)]
        tt("dve", modcol[:], ps[1][:, 0:48], col[:, C_BADA:C_BADA + 48], ALU.add, ["ps1", "col"], ["modcol"])
        stt(gcol[:, 0:8], modcol[:, 8:16], 1.0, col[:, C_N1G:C_N1G + 8], ALU.add, ALU.mult, ["modcol", "col"], ["gcol"])
        stt(gcol[:, 8:16], modcol[:, 32:40], 1.0, col[:, C_N2G:C_N2G + 8], ALU.add, ALU.mult, ["modcol", "col"],
            ["gcol"])

        bcslot = [BC[:, i * 2048:(i + 1) * 2048].bitcast(F32) for i in range(3)]
        dg = [dgt[:, 0:128], dgt[:, 128:256]]

        def expand(colsrc, slot, src_keys, pbank):
            for j in range(8):
                ts("dve", dg[j % 2], ident_f[:], colsrc[:, j:j + 1], None, ALU.mult, None,
                   ["ident_f"] + src_keys, [("dg", j % 2)])
                bank = ps[pbank + j // 4]
                mm(bank[:, (j % 4) * 128:(j % 4 + 1) * 128], ones_f[:], dg[j % 2], True, True,
                   [("dg", j % 2), "ones_f"], ["ps%d" % (pbank + j // 4)])
                if j % 4 == 3:
                    cp("act", bcslot[slot][:, (j // 4) * 512:(j // 4 + 1) * 512], bank[:, :],
                       ["ps%d" % (pbank + j // 4)], [("bc", slot)])

        expand(gcol[:, 0:8], 0, ["gcol"], 2)
        expand(modcol[:, 0:8], 1, ["modcol"], 2)

        fence([("slab", 0), ("slab", 1)] + [("hT", b_) for b_ in range(8)])
        if stop == 'p0':
            return finish()
        hT = U1.rearrange("p (b k c) -> p b k c", b=8, k=8)
        xs = [RA[:, i * 2048:(i + 1) * 2048].bitcast(F32) for i in range(2)]
        hb = [RA[:, 4096 + i * 1024:4096 + (i + 1) * 1024] for i in range(2)]
        psT = [ps[4][:].bitcast(BF16), ps[5][:].bitcast(BF16)]
        for t in range(32):
            i = t % 2
            blk, tc_ = t // 4, (t % 4) * 128
            dma("sp", xs[i], xp[t * 128:(t + 1) * 128, :], "xs%d" % i, [], [("xs", i)])
            act(hb[i], xs[i], AF.Square, [("xs", i)], [("hb", i), "stat"], accum_out=stat[:, t:t + 1])
            rsqrt_chain(stat[:, 32 + t:33 + t], stat[:, t:t + 1], 1.0 / 1024, ["stat"], ["stat"])
            stt(xs[i], xs[i], stat[:, 32 + t:33 + t], bcslot[0], ALU.mult, ALU.mult, [("xs", i), "stat", ("bc", 0)],
                [("xs", i)])
            tt("dve", hb[i], xs[i], bcslot[1], ALU.add, [("xs", i), ("bc", 1)], [("hb", i)])
            for k in range(8):
                tr(psT[i][:, k * 128:(k + 1) * 128], hb[i][:, k * 128:(k + 1) * 128], ident_b[:],
                   [("hb", i), "ident_b"], ["ps%d" % (4 + i)])
            cp("act", hT[:, blk, :, tc_:tc_ + 128], psT[i].rearrange("p (k c) -> p k c", k=8), ["ps%d" % (4 + i)],
               [("hT", blk)])

        if stop == 'A':
            return finish()
        C1 = RA[:, 0:4096]
        C2 = RA[:, 4096:8192]
        posi = HB[:, 0:8192].bitcast(I32)
        ang = HB[:, 0:8192].bitcast(F32)
        RA_KEYS = [("xs", 0), ("xs", 1), ("hb", 0), ("hb", 1)]
        fence(RA_KEYS + ["C1", "C2"])
        dma("sp", posi, posp[0, :].partition_broadcast(128), "posi", [], ["HB"])
        TWO_PI = 2.0 * math.pi
        CW1 = 6.28125
        CW2 = TWO_PI - CW1
        nint = bcslot[2].bitcast(I32)
        nflt = bcslot[2]
        for (Ct, shc, key) in ((C1, 1, "C1"), (C2, 2, "C2")):
            for q4 in range(4):
                sl = slice(q4 * 1024, (q4 + 1) * 1024)
                tmpf = MI[:, 0:2048].bitcast(F32)
                K1, K2 = ["mi_tmp"], [("bc", 2)]
                cp("dve", tmpf, posi[:, sl], ["HB"], K1)
                ts("dve", tmpf, tmpf, cstt[:, 0:1], cstt[:, shc:shc + 1], ALU.mult, ALU.add, K1 + ["cst"], K1)
                ts("dve", nflt, tmpf, 1.0 / TWO_PI, None, ALU.mult, None, K1, K2)
                cp("dve", nint, nflt, K2, K2)
                cp("dve", nflt, nint, K2, K2)
                stt(tmpf, nflt, -CW1, tmpf, ALU.mult, ALU.add, K1 + K2, K1)
                stt(tmpf, nflt, -CW2, tmpf, ALU.mult, ALU.add, K1 + K2, K1)
                ts("dve", nflt, tmpf, math.pi, -TWO_PI, ALU.is_gt, ALU.mult, K1, K2)
                tt("dve", tmpf, tmpf, nflt, ALU.add, K1 + K2, K1)
                ts("dve", nflt, tmpf, -math.pi, TWO_PI, ALU.is_lt, ALU.mult, K1, K2)
                tt("dve", tmpf, tmpf, nflt, ALU.add, K1 + K2, K1)
                ts("dve", tmpf, tmpf, math.pi, -math.pi, ALU.min, ALU.max, K1, K1)
                act(Ct[:, sl], tmpf, AF.Sin, K1, [key])

        if stop == 'rope':
            return finish()
        tt("dve", misc[:, 0:1], col[:, C_DQG:C_DQG + 1], col[:, C_DKG:C_DKG + 1], ALU.mult, ["col"], ["misc0"])
        tt("dve", misc[:, 4:5], col[:, C_LAM:C_LAM + 1], col[:, C_LAM + 1:C_LAM + 2], ALU.mult, ["col"], ["misc4"])
        tt("dve", misc[:, 5:6], col[:, C_LAM + 2:C_LAM + 3], col[:, C_LAM + 3:C_LAM + 4], ALU.mult, ["col"], ["misc4"])
        mm(ps[0][:, 0:2], ones_f[:], misc[:, 4:6], True, True, ["misc4", "ones_f"], ["ps0"])
        act(misc[:, 6:8], ps[0][:, 0:2], AF.Exp, ["ps0"], ["misc6"])
        stt(misc[:, 3:4], misc[:, 7:8], -LAMBDA_INIT, misc[:, 6:7], ALU.add, ALU.subtract, ["misc6"], ["misc3"])
        ts("dve", misc[:, 8:9], col[:, C_SUBLN:C_SUBLN + 1], 1.0 - LAMBDA_INIT, None, ALU.mult, None, ["col"],
           ["misc8"])

        Rt = MI[:, 2048:2048 + 512].bitcast(F32)
        p.op("pool", lambda e: e.iota(Rt, [[-1, 256]], base=0, channel_multiplier=1,
                                      allow_small_or_imprecise_dtypes=True), [], ["Rt"])
        biasT = [U2[:, 20480 + h * 512:20480 + (h + 1) * 512].bitcast(F32) for h in range(4)]
        relb3 = relb[:].rearrange("p (b h) -> p b h", h=4)
        neg_thr = [(1, 1), (2, 2), (3, 3), (4, 4), (5, 5), (6, 6), (7, 7), (8, 8), (12, 9), (16, 10), (23, 11),
                   (32, 12), (46, 13), (64, 14), (91, 15)]
        for h in range(4):
            ts("dve", biasT[h], Rt, 0.0, relb3[:, 0, h:h + 1], ALU.mult, ALU.add, ["Rt", "relb"], [("biasT", h)])
        prev_n, prev_p = 0, 0
        for (thr, b) in neg_thr:
            ts("dve", steps, Rt, float(-thr), None, ALU.is_le, None, ["Rt"], ["steps"])
            for h in range(4):
                tt("dve", misc[:, 10:11], relb3[:, b, h:h + 1], relb3[:, prev_n, h:h + 1], ALU.subtract, ["relb"],
                   ["misc10"])
                stt(biasT[h], steps, misc[:, 10:11], biasT[h], ALU.mult, ALU.add, ["steps", "misc10"],
                    [("biasT", h)])
            ts("dve", steps, Rt, float(thr), None, ALU.is_ge, None, ["Rt"], ["steps"])
            for h in range(4):
                tt("dve", misc[:, 10:11], relb3[:, 16 + b, h:h + 1], relb3[:, prev_p, h:h + 1], ALU.subtract,
                   ["relb"], ["misc10"])
                stt(biasT[h], steps, misc[:, 10:11], biasT[h], ALU.mult, ALU.add, ["steps", "misc10"],
                    [("biasT", h)])
            prev_n, prev_p = b, 16 + b
        for h in range(4):
            ts("dve", biasT[h], biasT[h], relb3[:, 15, h:h + 1], None, ALU.subtract, None,
               [("biasT", h), "relb"], [("biasT", h)])
            act(biasT[h], biasT[h], AF.Exp, [("biasT", h)], [("biasT", h)])
        btmps = [MI[:, 2560 + i * 256:2560 + (i + 1) * 256].bitcast(F32) for i in range(2)]

        if stop == 'bias':
            return finish()
        kT = HB[:, 0:4096]
        Vb = HB[:, 4096:8192]
        qT = HB[:, 8192:10240]
        qTb = BC[:, 4096:6144]
        attnT = AT.rearrange("p (c n) -> p c n", c=8)
        pT = [MI[:, 3072 + i * 512:3072 + (i + 1) * 512] for i in range(2)]
        pT2 = [MI[:, 0:512], MI[:, 512:1024]]
        sqb = MI[:, 1024:1536]
        rsf = MI[:, 1536:2560].bitcast(F32)
        tA = RA[:, 0:1]
        PJ_PA, PJ_NB = [0, 1, 4, 5], [2, 7]
        rsfs = [rsf, MI[:, 3072:4096].bitcast(F32)]
        rsfk = [["rsf"], [("pT", 0), ("pT", 1)]]

        def key_tiles(g):
            res = []
            for j in range(0, 4 * g + 4):
                c0 = 128 * max(0, j - 4 * g)
                res.append((16 + j, c0, "oth", j))
                res.append((j, c0, "own", j))
            return res

        PBUF = [pT[0], pT[1], pT2[0], pT2[1]]

        def attention(h, kind):
            diff = kind == "diff"
            nsub = 2 if diff else 1
            sbanks = [0, 1, 2, 3] if diff else [0, 1, 5, 6]
            scale = (64 ** -0.5) if diff else (96 ** -0.5)
            LOOK = 3
            items = []
            for g in range(4):
                tiles = key_tiles(g)
                for ti, (kt, c0, knd, j) in enumerate(tiles):
                    for sub in range(nsub):
                        items.append((g, ti, len(tiles), kt, c0, knd, j, sub))

            def stage1(idx):
                g, ti, nt, kt, c0, knd, j, sub = items[idx]
                slot = idx % 4
                bank, bkey = ps[sbanks[slot]], "ps%d" % sbanks[slot]
                pbuf, pkey = PBUF[slot], ("pT", slot)
                ncol = 512 - c0
                kc = slice(kt * 128, (kt + 1) * 128)
                qcols = slice(g * 512 + c0, (g + 1) * 512)
                in_group = j >= 4 * g
                qsrc = (qT, qTb)[sub] if diff else qT
                need_bias = diff and in_group
                need_bias2 = diff and knd == "own" and 4 * g <= j + 1 < 4 * g + 4
                mm(bank[:, 0:ncol], kT[:, kc], qsrc[:, qcols], True, True, ["kT", "qT"], [bkey])
                if knd == "oth" and in_group:
                    act(pbuf[:, 0:128], bank[:, 0:128], AF.Exp, [bkey, "mb"], [pkey], scale=scale,
                        bias=mbt[:, j:j + 1])
                    if ncol > 128:
                        act(pbuf[:, 128:ncol], bank[:, 128:ncol], AF.Exp, [bkey], [pkey], scale=scale)
                else:
                    act(pbuf[:, 0:ncol], bank[:, 0:ncol], AF.Exp, [bkey], [pkey], scale=scale)
                if need_bias:
                    bsl = biasT[h][:, 0:128] if knd == "own" else biasT[h][:, 128:256]
                    tt("pool", pbuf[:, 0:128], pbuf[:, 0:128], bsl, ALU.mult, [pkey, ("biasT", h)], [pkey])
                if need_bias2:
                    bt = btmps[(j + 1) % 2]
                    if sub == 0:
                        ts("dve", bt, biasT[h][:, 128:256], mbt[:, 16 + j + 1:16 + j + 2],
                           mbt[:, 32 + j + 1:32 + j + 2], ALU.mult, ALU.add,
                           [("biasT", h), "mb"], [("btmp", (j + 1) % 2)])
                    cs = 128 * (j + 1 - 4 * g) - c0
                    tt("pool", pbuf[:, cs:cs + 128], pbuf[:, cs:cs + 128], bt, ALU.mult,
                       [pkey, ("btmp", (j + 1) % 2)], [pkey])
                if knd == "own" and in_group:
                    memset("pool", pbuf[64:128, 0:64], 0.0, [], [pkey])

            def stage2(idx):
                g, ti, nt, kt, c0, knd, j, sub = items[idx]
                slot = idx % 4
                pbuf, pkey = PBUF[slot], ("pT", slot)
                ncol = 512 - c0
                kc = slice(kt * 128, (kt + 1) * 128)
                first, last = ti == 0, ti == nt - 1
                if diff:
                    ob, okey = ps[4 + sub], "ps%d" % (4 + sub)
                    mm(ob[:, c0:512], Vb[:, kc], pbuf[:, 0:ncol], first, last, ["V_h", pkey], [okey])
                    db, dkey = ps[6 + sub], "ps%d" % (6 + sub)
                    mm(db[:, c0:512], ones_b[:], pbuf[:, 0:ncol], first, last, ["ones_b", pkey], [dkey])
                else:
                    ob, okey = ps[2 + g % 2], "ps%d" % (2 + g % 2)
                    mm(ob[:, c0:512], Vb[:, kc], pbuf[:, 0:ncol], first, last, ["V_h", pkey], [okey])
                if last and sub == nsub - 1:
                    finalize(g)

            def finalize(g):
                qs = slice(g * 512, (g + 1) * 512)
                s_a = bcslot[0][:, 0:512]
                s_b = bcslot[0][:, 512:1024]
                s_c = bcslot[1][:, 0:512]
                s_d = bcslot[1][:, 512:1024]
                rhi = BC[:, 2048:2560]
                rlo = BC[:, 2560:3072]
                if diff:
                    cp("dve", s_a, ps[6][:, :], ["ps6"], ["s_a"])
                    cp("dve", s_b, ps[7][:, :], ["ps7"], ["s_b"])
                    cp("dve", s_c, ps[4][:, :], ["ps4"], ["s_c"])
                    cp("dve", s_d, ps[5][:, :], ["ps5"], ["s_d"])
                    recip(s_a, s_a, ["s_a"], ["s_a"])
                    recip(s_b, s_b, ["s_b"], ["s_b"])
                    tt("dve", s_a, s_c, s_a, ALU.mult, ["s_c", "s_a"], ["s_a"])
                    stt(s_b, s_d, misc[:, 3:4], s_b, ALU.mult, ALU.mult, ["s_d", "s_b", "misc3"], ["s_b"])
                    tt("dve", s_a, s_a, s_b, ALU.add, ["s_a", "s_b"], ["s_a"])
                    tt("dve", sqb, s_a, s_a, ALU.mult, ["s_a"], ["sqb"])
                    def fin_b_diff(idx_, s_a=s_a, s_c=s_c, qs=qs, g=g):
                        bn = sbanks[idx_ % 4]
                        mm(ps[bn][:, :], ones_b[:], sqb, True, True, ["sqb", "ones_b"], ["ps%d" % bn])
                        rsqrt_chain(s_c, ps[bn][:, :], 1.0 / 128, ["ps%d" % bn], ["s_c"])
                        stt(attnT[:, 4 + h, qs], s_a, misc[:, 8:9], s_c, ALU.mult, ALU.mult,
                            ["s_a", "s_c", "misc8"], [("at", g)])
                    pending.append([8, fin_b_diff])
                else:
                    ob, okey = ps[2 + g % 2], "ps%d" % (2 + g % 2)
                    r0 = 64 if h % 2 == 0 else 0
                    rhi = BC[:, 2048:2560] if h % 2 == 0 else BC[:, 3072:3584]
                    rlo = BC[:, 2560:3072] if h % 2 == 0 else BC[:, 3584:4096]
                    o0 = 0 if h % 2 == 0 else 64
                    rr, orows = slice(r0, r0 + 1), slice(o0, o0 + 64)
                    recip(s_a[rr, :], ob[rr, :], [okey], ["s_a"])
                    cp("dve", rhi[rr, :], s_a[rr, :], ["s_a"], ["s_c"])
                    tt("dve", s_a[rr, :], s_a[rr, :], rhi[rr, :], ALU.subtract, ["s_a", "s_c"], ["s_a"])
                    cp("dve", rlo[rr, :], s_a[rr, :], ["s_a"], ["s_c"])
                    def fin_b(idx_, rhi=rhi, rlo=rlo, orows=orows, ob=ob, okey=okey, qs=qs, s_b=s_b, g=g):
                        mm(ps[4][:, :], ones_b[:, :], rhi[:, :], True, False, ["s_c", "ones_b"], ["ps4"])
                        mm(ps[4][:, :], ones_b[:, :], rlo[:, :], False, True, ["s_c", "ones_b"], ["ps4"])
                        cp("act", s_b[orows, :], ps[4][orows, :], ["ps4"], ["s_b"])
                        tt("dve", attnT[orows, h // 2, qs], ob[orows, :], s_b[orows, :], ALU.mult, [okey, "s_b"],
                           [("at", g)])
                    pending.append([6, fin_b])

            n = len(items)
            pending = []
            for idx in range(n + LOOK):
                if idx < n:
                    stage1(idx)
                if idx - LOOK >= 0:
                    stage2(idx - LOOK)
                for pe_ in list(pending):
                    pe_[0] -= 1
                    if pe_[0] <= 0:
                        pending.remove(pe_)
                        pe_[1](idx)
            for pe_ in pending:
                pe_[1](n + LOOK - 1)

        HTK = [("hT", b) for b in range(8)]

        fence(["HB", "kT", "qT", "V_h"])
        fence(["mi_tmp", "Rt", "rsf", "sqb"] + [("pT", a_) for a_ in range(4)])
        fence([("bc", 0), ("bc", 1), ("bc", 2), "s_a", "s_b", "s_c", "s_d", "t1", "t2"])
        memset("pool", qT[64:128, :], 0.0, [], ["qT"])
        memset("pool", qTb[0:64, :], 0.0, [], ["qT", ("bc", 2), "t1", "t2"])
        Vd = Vb.rearrange("p (t e) -> p t e", t=32)
        for h in range(4):
            cq0, ck0, cv0 = 672 + h * 128, 1184 + h * 128, 1696 + h * 128
            jobs = [("k", ck0, b_, kT, "kT") for b_ in range(8)] + [("q", cq0, b_, qT, "qT") for b_ in range(4)]

            def b_part1(i):
                side, c0w, blk, dst, dkey = jobs[i]
                pa, pak = ps[PJ_PA[i % 4]], "ps%d" % PJ_PA[i % 4]
                nb, nbk = ps[PJ_NB[i % 2]], "ps%d" % PJ_NB[i % 2]
                for k in range(8):
                    mm(pa[:, :], win[:, k, c0w:c0w + 128], hT[:, blk, k, :], k == 0, k == 7,
                       [("win", k), ("hT", blk)], [pak])

            def b_part1b(i):
                pa, pak = ps[PJ_PA[i % 4]], "ps%d" % PJ_PA[i % 4]
                nb, nbk = ps[PJ_NB[i % 2]], "ps%d" % PJ_NB[i % 2]
                act(sqb, pa[:, :], AF.Square, [pak], ["sqb"])
                mm(nb[:, :], blk64[:], sqb, True, True, ["sqb", "blk64"], [nbk])

            def b_part2(i):
                side, c0w, blk, dst, dkey = jobs[i]
                bs = slice(blk * 512, (blk + 1) * 512)
                pa, pak = ps[PJ_PA[i % 4]], "ps%d" % PJ_PA[i % 4]
                nb, nbk = ps[PJ_NB[i % 2]], "ps%d" % PJ_NB[i % 2]
                rs, rsk = rsfs[i % 2], rsfk[i % 2]
                rsqrt_chain(rs, nb[:, :], 1.0 / 64, [nbk], rsk)
                if side == "k":
                    stt(dst[:, bs], pa[:, :], misc[:, 0:1], rs, ALU.mult, ALU.mult, [pak, "misc0"] + rsk, [dkey])
                else:
                    tt("dve", qT[0:64, bs], pa[0:64, :], rs[0:64, :], ALU.mult, [pak] + rsk, [dkey])
                    tt("dve", qTb[64:128, bs], pa[64:128, :], rs[64:128, :], ALU.mult, [pak] + rsk, [dkey])

            b_part1(0)
            b_part1(1)
            for i in range(len(jobs) + 1):
                if i < len(jobs):
                    b_part1b(i)
                if i + 2 < len(jobs):
                    b_part1(i + 2)
                if i >= 1:
                    b_part2(i - 1)
            for t in range(32):
                blk, tc_ = t // 4, (t % 4) * 128
                vbn = 3 if (t // 4) % 2 == 0 else 6
                for k in range(8):
                    mm(ps[vbn][:, (t % 4) * 128:(t % 4 + 1) * 128], hT[:, blk, k, tc_:tc_ + 128],
                       win[:, k, cv0:cv0 + 128], k == 0, k == 7, [("win", k), ("hT", blk)], ["ps%d" % vbn])
                if t % 4 == 3:
                    cp("act", Vd[:, t - 3:t + 1, :], ps[vbn][:, :].rearrange("p (t e) -> p t e", t=4), ["ps%d" % vbn],
                       ["V_h"])
            attention(h, "diff")

        if stop == 'B':
            return finish()
        U1f = U1
        fence(["qT", ("bc", 2), "t1", "t2"])

        def blkv(blk, off, n):
            return U1f[:, blk * 4096 + off:blk * 4096 + off + n]

        import os as _os
        for blk in range(int(_os.environ.get('KBLK', '8'))):
            own = blk < 4
            banks = {}
            for m in range(2):
                for k in range(8):
                    mm(ps[m][:, :], win[:, k, 384 + m * 128:384 + (m + 1) * 128], hT[:, blk, k, :], k == 0, k == 7,
                       [("win", k), ("hT", blk)], ["ps%d" % m])
            if own:
                for m in range(3):
                    for k in range(8):
                        mm(ps[2 + m][:, :], win[:, k, m * 128:(m + 1) * 128], hT[:, blk, k, :], k == 0, k == 7,
                           [("win", k), ("hT", blk)], ["ps%d" % (2 + m)])
            for k in range(8):
                mm(ps[5][0:96, :], kpeW3[:, k, :], hT[:, blk, k, :], k == 0, k == 7, ["kpeW", ("hT", blk)], ["ps5"])
            for k in range(8):
                mm(ps[6][0:96, :], kpeWr3[:, k, :], hT[:, blk, k, :], k == 0, k == 7, ["kpeWr", ("hT", blk)], ["ps6"])
            if stop == 'Ca':
                return finish()
            hk = ("hT", blk)
            for m in range(2):
                act(sqb, ps[m][:, :], AF.Square, ["ps%d" % m], ["sqb"])
                mm(ps[7][:, :], ones_b[:], sqb, m == 0, m == 1, ["sqb", "ones_b"], ["ps7"])
            rsqrt_chain(rsf, ps[7][:, :], 1.0 / 256, ["ps7"], ["rsf"])
            for m in range(2):
                stt(blkv(blk, m * 512, 512), ps[m][:, :], col[:, C_CKVG + m:C_CKVG + m + 1], rsf, ALU.mult, ALU.mult,
                    ["ps%d" % m, "rsf", "col"], [hk])
            if stop == 'Cb':
                return finish()
            if own:
                for m in range(3):
                    act(sqb, ps[2 + m][:, :], AF.Square, ["ps%d" % (2 + m)], ["sqb"])
                    mm(ps[7][:, :], ones_b[:], sqb, m == 0, m == 2, ["sqb", "ones_b"], ["ps7"])
                rsqrt_chain(rsf, ps[7][:, :], 1.0 / 384, ["ps7"], ["rsf"])
                for m in range(3):
                    stt(blkv(blk, 1024 + m * 512, 512), ps[2 + m][:, :], col[:, C_CQG + m:C_CQG + m + 1], rsf,
                        ALU.mult, ALU.mult, ["ps%d" % (2 + m), "rsf", "col"], [hk])
            if stop == 'Cc':
                return finish()
            _sk = _os.environ.get('KSKIP', '')
            if 'a' not in _sk:
                act(blkv(blk, 3072, 512)[0:96, :], ps[5][0:96, :], AF.Square, ["ps5"], [hk])
                memset("pool", blkv(blk, 3072, 512)[96:128, :], 0.0, [], [hk])
            bsl = slice(blk * 512, (blk + 1) * 512)
            t1 = bcslot[2][:, 0:512]
            t2 = bcslot[2][:, 512:1024]
            if 'b' not in _sk:
                stt(t1[:, :], ps[5][:, :], col[:, C_KG:C_KG + 1], C1[:, bsl], ALU.mult, ALU.mult,
                    ["ps5", "col", "C1"], ["t1"])
            if 'c' not in _sk:
                stt(t2[:, :], ps[6][:, :], col[:, C_KGR:C_KGR + 1], C2[:, bsl], ALU.mult, ALU.mult,
                    ["ps6", "col", "C2"], ["t2"])
            if 'd' not in _sk:
                tt("dve", blkv(blk, 2560, 512)[:, :], t1[:, :], t2[:, :], ALU.add, ["t1", "t2"], [hk])

        if stop == 'Cd' :
            return finish()
        if stop == 'C1':
            return finish()
        wuq = U2[:, 0:2304].rearrange("p (k n) -> p k n", k=3)
        wuqr = U2[:, 2304:4608].rearrange("p (k n) -> p k n", k=3)
        wukv = U2[:, 4608:6656].rearrange("p (k n) -> p k n", k=2)
        w_uq_v = w_uq.rearrange("(k p) n -> p k n", p=128)
        w_uq_v4 = w_uq.rearrange("(k p) (h d) -> p k h d", p=128, d=96)
        wuqr4 = U2[:, 2304:4608].rearrange("p (k h d) -> p k h d", k=3, d=96)
        dma("pool", wuq, w_uq_v, "wuq", [], WIN)
        memset("dve", U2[:, 2304:4608], 0.0, [], WIN)
        for k in range(3):
            dma("pool", wuqr4[:, k, :, 64:80], w_uq_v4[:, k, :, 80:96], "wuqr", [], WIN)
            dma("pool", wuqr4[:, k, :, 80:96], w_uq_v4[:, k, :, 64:80], "wuqr", [], WIN)
        dma("pool", wukv, w_ukv.rearrange("(k p) n -> p k n", p=128), "wukv", [], WIN)
        wo = U2[:, 8192:8192 + 8192].rearrange("p (k n) -> p k n", k=8)
        dma("pool", wo, w_o.rearrange("(k p) n -> p k n", p=128), "wo", [], WIN)

        if stop == 'C':
            return finish()
        Vm = Vb.rearrange("p (t e) -> p t e", t=32)
        memset("pool", kT[96:128, :], 0.0, [], ["kT"])
        sqk = MI[:, 2560:3072]
        memset("pool", sqk, 0.0, [], [("btmp", 0), ("btmp", 1), "sqk"])
        memset("pool", sqb[96:128, :], 0.0, [], ["sqb"])
        memset("pool", BC[:, 2048:4096], 0.0, [], ["s_c", "s_d", ("bc", 1)])
        for h in range(8):
            even = h % 2 == 0
            memset("pool", Vb, 0.0, [], ["V_h"])
            memset("pool", Vm[:, :, 64:65] if even else Vm[:, :, 0:1], 1.0, [], ["V_h"])
            def k_part1(blk):
                hk = ("hT", blk)
                pa, pak = ps[PJ_PA[blk % 4]], "ps%d" % PJ_PA[blk % 4]
                nb, nbk = ps[PJ_NB[blk % 2]], "ps%d" % PJ_NB[blk % 2]
                for m in range(2):
                    mm(pa[:, :], wukv[:, m, h * 128:(h + 1) * 128], blkv(blk, m * 512, 512), m == 0, m == 1,
                       WIN + [hk], [pak])

            def k_part1b(blk):
                hk = ("hT", blk)
                pa, pak = ps[PJ_PA[blk % 4]], "ps%d" % PJ_PA[blk % 4]
                nb, nbk = ps[PJ_NB[blk % 2]], "ps%d" % PJ_NB[blk % 2]
                act(sqk[0:64, :], pa[0:64, :], AF.Square, [pak], ["sqk"])
                mm(nb[:, :], ones_b[:, :], sqk[:, :], True, False, ["sqk", "ones_b"], [nbk])
                mm(nb[:, :], ones_b[:, :], blkv(blk, 3072, 512)[:, :], False, True, [hk, "ones_b"], [nbk])

            def k_part2(blk):
                hk = ("hT", blk)
                bs = slice(blk * 512, (blk + 1) * 512)
                pa, pak = ps[PJ_PA[blk % 4]], "ps%d" % PJ_PA[blk % 4]
                nb, nbk = ps[PJ_NB[blk % 2]], "ps%d" % PJ_NB[blk % 2]
                rs, rsk = rsfs[blk % 2], rsfk[blk % 2]
                rsqrt_chain(rs[0:96, :], nb[0:96, :], 1.0 / 96, [nbk], rsk, pr=(0, 96))
                stt(kT[0:64, bs], pa[0:64, :], col[0:64, C_KG:C_KG + 1], rs[0:64, :], ALU.mult, ALU.mult,
                    [pak, "col"] + rsk, ["kT"])
                tt("dve", kT[64:96, bs], blkv(blk, 2560, 512)[64:96, :], rs[64:96, :], ALU.mult, [hk] + rsk, ["kT"])

            k_part1(0)
            k_part1(1)
            for i in range(9):
                if i < 8:
                    k_part1b(i)
                if i + 2 < 8:
                    k_part1(i + 2)
                if i >= 1:
                    k_part2(i - 1)
            for t in range(32):
                blk, tc_ = t // 4, (t % 4) * 128
                vbn = 3 if (t // 4) % 2 == 0 else 6
                for m in range(2):
                    mm(ps[vbn][:, (t % 4) * 64:(t % 4 + 1) * 64], blkv(blk, m * 512, 512)[:, tc_:tc_ + 128],
                       wukv[:, m, h * 128 + 64:h * 128 + 128], m == 0, m == 1, WIN + [("hT", blk)], ["ps%d" % vbn])
                if t % 4 == 3:
                    dstv = Vm[:, t - 3:t + 1, 0:64] if even else Vm[:, t - 3:t + 1, 64:128]
                    cp("act", dstv, ps[vbn][:, 0:256].rearrange("p (t e) -> p t e", t=4), ["ps%d" % vbn], ["V_h"])
            QB = [(0, 1), (4, 5)]

            def q_part1(blk):
                hk = ("hT", blk)
                nb, nbk = ps[PJ_NB[blk % 2]], "ps%d" % PJ_NB[blk % 2]
                for (bank, wbase) in ((QB[blk % 2][0], 0), (QB[blk % 2][1], 2304)):
                    for m in range(3):
                        w0 = wbase + m * 768 + h * 96
                        mm(ps[bank][:, :], U2[:, w0:w0 + 128], blkv(blk, 1024 + m * 512, 512),
                           m == 0, m == 2, WIN + [hk], ["ps%d" % bank])
                ba = QB[blk % 2][0]
                act(sqb[0:96, :], ps[ba][0:96, :], AF.Square, ["ps%d" % ba], ["sqb"])
                mm(nb[:, :], ones_b[:, :], sqb[:, :], True, True, ["sqb", "ones_b"], [nbk])

            def q_part2(blk):
                bs = slice(blk * 512, (blk + 1) * 512)
                nb, nbk = ps[PJ_NB[blk % 2]], "ps%d" % PJ_NB[blk % 2]
                ba, bb = QB[blk % 2]
                rs, rsk = rsfs[blk % 2], rsfk[blk % 2]
                rsqrt_chain(rs[0:96, :], nb[0:96, :], 1.0 / 96, [nbk], rsk, pr=(0, 96))
                t1 = bcslot[2][:, 0:512]
                t2 = bcslot[2][:, 512:1024]
                stt(t1[0:96, :], ps[ba][0:96, :], col[0:96, C_QG:C_QG + 1], C1[0:96, bs], ALU.mult, ALU.mult,
                    ["ps%d" % ba, "col", "C1"], ["t1"])
                stt(t2[0:96, :], ps[bb][0:96, :], col[0:96, C_QGR:C_QGR + 1], C2[0:96, bs], ALU.mult, ALU.mult,
                    ["ps%d" % bb, "col", "C2"], ["t2"])
                tt("dve", t1[0:96, :], t1[0:96, :], t2[0:96, :], ALU.add, ["t1", "t2"], ["t1"])
                tt("dve", qT[0:96, bs], t1[0:96, :], rs[0:96, :], ALU.mult, ["t1"] + rsk, ["qT"])

            for i in range(5):
                if i < 4:
                    q_part1(i)
                if i >= 1:
                    q_part2(i - 1)
            attention(h, "mla")

        if stop == 'D':
            return finish()
        ATK = [("at", g) for g in range(4)]
        fence([("bc", 0), ("bc", 1), ("bc", 2), "s_a", "s_b", "s_c", "s_d", "t1", "t2"])
        fence(["C1", "C2", "xs2", ("h2f", 0), ("h2f", 1), "h2Tf"])
        expand(modcol[:, 16:24], 0, ["modcol"], 6)
        expand(gcol[:, 8:16], 1, ["gcol"], 6)
        expand(modcol[:, 24:32], 2, ["modcol"], 6)
        x1 = U1.bitcast(F32).rearrange("p (s n) -> p s n", s=16)
        h2T = attnT
        xs2 = RA[:, 0:2048].bitcast(F32)
        h2f = RA[:, 2048:4096].bitcast(F32)
        h2Tf = RA[:, 4096:6144].bitcast(F32).rearrange("p (k c) -> p k c", k=8)
        gate3 = gate[:].rearrange("p (s e) -> p s e", s=16)
        wr3 = wr_f[:].rearrange("p (k n) -> p k n", k=8)
        h2fs = [RA[:, 2048:4096].bitcast(F32), RA[:, 6144:8192].bitcast(F32)]

        def e_part1(s):
            h2f, h2fk = h2fs[s % 2], ("h2f", s % 2)
            sc = slice(s * 128, (s + 1) * 128)
            hk = ("hT", s // 2)
            dma("sp", xs2, xp[s * 128:(s + 1) * 128, :], "xs2", [], ["xs2"])
            wob = (0, 1) if s % 2 == 0 else (5, 6)
            for half in range(2):
                for c in range(8):
                    mm(ps[wob[half]][:, :], attnT[:, c, sc], wo[:, c, half * 512:(half + 1) * 512], c == 0, c == 7,
                       ATK + WIN, ["ps%d" % wob[half]])
            for half in range(2):
                hs = slice(half * 512, (half + 1) * 512)
                tt("dve", x1[:, s, hs], ps[wob[half]][:, :], bcslot[0][:, hs], ALU.mult,
                   ["ps%d" % wob[half], ("bc", 0)], [hk, ("x1", s)])
                tt("dve", x1[:, s, hs], x1[:, s, hs], xs2[:, hs], ALU.add, ["xs2", ("x1", s)], [("x1", s)])
            act(h2f, x1[:, s, :], AF.Square, [("x1", s)], [h2fk, "stat2"], accum_out=stat2[:, s:s + 1])
            rsqrt_chain(stat2[:, 32 + s:33 + s], stat2[:, s:s + 1], 1.0 / 1024, ["stat2"], ["stat2"])
            stt(h2f, x1[:, s, :], stat2[:, 32 + s:33 + s], bcslot[1], ALU.mult, ALU.mult,
                [("x1", s), "stat2", ("bc", 1)], [h2fk])
            tt("dve", h2f, h2f, bcslot[2], ALU.add, [h2fk, ("bc", 2)], [h2fk])

        def e_part2(s):
            h2f, h2fk = h2fs[s % 2], ("h2f", s % 2)
            sc = slice(s * 128, (s + 1) * 128)
            for k in range(8):
                tr(ps[2 + k // 4][:, (k % 4) * 128:(k % 4 + 1) * 128], h2f[:, k * 128:(k + 1) * 128], ident_f[:],
                   [h2fk, "ident_f"], ["ps%d" % (2 + k // 4)])
            for hf in range(2):
                cp("act", h2T[:, hf * 4:(hf + 1) * 4, sc], ps[2 + hf][:, :].rearrange("p (k c) -> p k c", k=4),
                   ["ps%d" % (2 + hf)], ATK + [("h2T", s)])
                cp("dve", h2Tf[:, hf * 4:(hf + 1) * 4, :], ps[2 + hf][:, :].rearrange("p (k c) -> p k c", k=4),
                   ["ps%d" % (2 + hf)], ["h2Tf"])
            for k in range(8):
                mm(ps[4][:, 0:20], h2Tf[:, k, :], wr3[:, k, :], k == 0, False, ["h2Tf", "wr"], ["ps4"])
            mm(ps[4][:, 0:20], ones_f[0:1, :], br_f[0:1, :], False, True, ["ones_f", "br"], ["ps4"])
            R = rt
            cp("dve", R[:, 0:20], ps[4][:, 0:20], ["ps4"], ["rt"])
            RK = ["rt"]
            rmax(R[:, 20:21], R[:, 0:4], RK, RK)
            ts("dve", R[:, 24:28], R[:, 0:4], R[:, 20:21], None, ALU.is_equal, None, RK, RK)
            ts("dve", R[:, 21:22], R[:, 20:21], -1.0, None, ALU.mult, None, RK, RK)
            act(R[:, 28:32], R[:, 0:4], AF.Exp, RK, RK, bias=R[:, 21:22], accum_out=R[:, 22:23])
            recip(R[:, 23:24], R[:, 22:23], RK, RK)
            ts("dve", R[:, 32:36], R[:, 4:8], R[:, 24:25], None, ALU.mult, None, RK, RK)
            for g_ in range(1, 4):
                stt(R[:, 32:36], R[:, 4 + 4 * g_:8 + 4 * g_], R[:, 24 + g_:25 + g_], R[:, 32:36], ALU.mult, ALU.add,
                    RK, RK)
            rmax(R[:, 36:37], R[:, 32:36], RK, RK)
            ts("dve", R[:, 40:44], R[:, 32:36], R[:, 36:37], None, ALU.is_equal, None, RK, RK)
            stt(R[:, 44:48], R[:, 40:44], -1e30, R[:, 32:36], ALU.mult, ALU.add, RK, RK)
            rmax(R[:, 37:38], R[:, 44:48], RK, RK)
            ts("dve", R[:, 48:52], R[:, 44:48], R[:, 37:38], None, ALU.is_equal, None, RK, RK)
            tt("dve", R[:, 38:39], R[:, 37:38], R[:, 36:37], ALU.subtract, RK, RK)
            act(R[:, 39:40], R[:, 38:39], AF.Exp, RK, RK)
            ts("dve", R[:, 52:53], R[:, 39:40], 1.0, None, ALU.add, None, RK, RK)
            recip(R[:, 53:54], R[:, 52:53], RK, RK)
            tt("dve", R[:, 53:54], R[:, 53:54], R[:, 23:24], ALU.mult, RK, RK)
            tt("dve", R[:, 54:55], R[:, 53:54], R[:, 39:40], ALU.mult, RK, RK)
            ts("dve", R[:, 56:60], R[:, 40:44], R[:, 53:54], None, ALU.mult, None, RK, RK)
            stt(R[:, 56:60], R[:, 48:52], R[:, 54:55], R[:, 56:60], ALU.mult, ALU.add, RK, RK)
            for g_ in range(4):
                ts("dve", gate3[:, s, 4 * g_:4 * g_ + 4], R[:, 56:60], R[:, 24 + g_:25 + g_], None, ALU.mult, None,
                   RK, ["gate"])


        e_part1(0)
        for s in range(16):
            if s + 1 < 16:
                e_part1(s + 1)
            e_part2(s)
        if stop == 'E':
            return finish()
        expand(modcol[:, 40:48], 0, ["modcol"], 6)
        fence(WIN + [(("wb", i_), t_) for i_ in range(2) for t_ in "gud"] + [("wb", 0), ("wb", 1)])
        fence(["kT", "qT", "V_h", "HB", "wds"])
        fence(["mi_tmp", "Rt", "rsf", "sqb", "sqk", ("btmp", 0), ("btmp", 1), "sgt"] + [("pT", a_) for a_ in range(4)]
              + [("biasT", h_) for h_ in range(4)] + [("heT", j_) for j_ in range(4)])
        wbuf = [U2[:, i * 12288:(i + 1) * 12288] for i in range(2)]
        wd_stage = HB[:, 0:8192].bitcast(F32).rearrange("p (j n) -> p j n", j=4)
        heT = [MI[:, i * 512:(i + 1) * 512] for i in range(4)]
        sgt = MI[:, 2048:3072].bitcast(F32)
        H2K = [("h2T", s) for s in range(16)]
        for e in range(N_EXP_RUN):
            wb = wbuf[e % 2]
            wg = wb[:, 0:4096].rearrange("p (k n) -> p k n", k=8)
            wu = wb[:, 4096:8192].rearrange("p (k n) -> p k n", k=8)
            wd = wb[:, 8192:12288].rearrange("p (j n) -> p j n", j=4)
            wk = ("wb", e % 2)
            dma("pool", wg, w_gate[e].rearrange("(k p) n -> p k n", p=128), "wg%d" % (e % 2), [], [wk, (wk, "g")])
            dma("pool", wu, w_up[e].rearrange("(k p) n -> p k n", p=128), "wu%d" % (e % 2), [], [(wk, "u")])
            dma("sp", wd_stage, w_down[e].rearrange("(j p) n -> p j n", p=128), "wds", [], ["wds"])
            for j in range(4):
                tt("pool", wd[:, j, :], wd_stage[:, j, :], bcslot[0], ALU.mult, ["wds", ("bc", 0)], [(wk, "d")])
            for blk in range(4):
                bs = slice(blk * 512, (blk + 1) * 512)
                for j in range(4):
                    for k in range(8):
                        mm(ps[0 + j % 2][:, :], wg[:, k, j * 128:(j + 1) * 128], h2T[:, k, bs], k == 0, k == 7,
                           H2K + [wk, (wk, "g")], ["ps%d" % (j % 2)])
                    for k in range(8):
                        mm(ps[2 + j % 2][:, :], wu[:, k, j * 128:(j + 1) * 128], h2T[:, k, bs], k == 0, k == 7,
                           H2K + [(wk, "u")], ["ps%d" % (2 + j % 2)])
                    act(sgt, ps[j % 2][:, :], AF.Silu, ["ps%d" % (j % 2)], ["sgt"])
                    tt("dve", heT[j], sgt, ps[2 + j % 2][:, :], ALU.mult, ["sgt", "ps%d" % (2 + j % 2)], [("heT", j)])
                for tt_ in range(4):
                    s = blk * 4 + tt_
                    for half in range(2):
                        bank = 4 + (tt_ * 2 + half) % 4
                        for j in range(4):
                            mm(ps[bank][:, :], heT[j][:, tt_ * 128:(tt_ + 1) * 128], wd[:, j, half * 512:(half + 1) * 512],
                               j == 0, j == 3, [("heT", j), (wk, "d")], ["ps%d" % bank])
                        hs = slice(half * 512, (half + 1) * 512)
                        stt(x1[:, s, hs], ps[bank][:, :], gate3[:, s, e:e + 1], x1[:, s, hs], ALU.mult, ALU.add,
                            ["ps%d" % bank, "gate", ("x1", s)], [("x1", s)])
        return finish()


def _own_tiles(half):
    return [j for j in range(32) if ((j % 4) in (0, 3)) == (half == 0)]


_NC_CACHE = {}


def kernel(**inputs):
    x = np.ascontiguousarray(inputs["x"], dtype=np.float32)
    pos = np.asarray(inputs["positions"]).astype(np.int32)
    f = lambda k: np.ascontiguousarray(np.asarray(inputs[k], dtype=np.float32)[0])
    inv_freq = (10000.0 ** (-np.arange(0, 16, dtype=np.float32) / 16.0)).astype(np.float32)
    cst = np.zeros((128, 8), np.float32)
    cst[:, 1] = 0.5 * math.pi
    cst[:, 2] = 0.0
    for i in range(16):
        cst[64 + i, 0] = inv_freq[i]
        cst[80 + i, 0] = inv_freq[i]
        cst[64 + i, 2] = math.pi
    w_r = np.ascontiguousarray(np.concatenate([f("w_rg"), f("w_re")], axis=1))
    b_r = np.ascontiguousarray(np.concatenate([f("b_rg"), f("b_re")])[None, :])
    in_maps = []
    metas = []
    for c in range(8):
        b, half = c // 2, c % 2
        own = _own_tiles(half)
        oth = [j for j in range(32) if j not in own]
        order = own + oth
        xt = x[b].reshape(32, 128, 1024)[order].reshape(4096, 1024)
        pt = pos[b].reshape(32, 128)[order].reshape(1, 4096)
        V = np.zeros((96, 128), np.float32)
        V[0:8] = np.asarray(inputs["c"], np.float32)[b].reshape(8, 128)
        V[8:56] = f("b_ada").reshape(48, 128)
        V[56:64] = f("norm1_g").reshape(8, 128)
        V[64:72] = f("norm2_g").reshape(8, 128)
        V[72:75] = f("mla_cq_g").reshape(3, 128)
        V[75:77] = f("mla_ckv_g").reshape(2, 128)
        V[77, 0:96] = f("mla_q_g")
        V[78, 0:96] = f("mla_k_g")
        V[79, 0:64] = f("diff_q_g"); V[79, 64:128] = f("diff_q_g")
        V[80, 0:64] = f("diff_k_g"); V[80, 64:128] = f("diff_k_g")
        V[81, :] = f("diff_subln_g")
        V[82, 0:64] = f("lambda_q1"); V[83, 0:64] = f("lambda_k1")
        V[84, 0:64] = f("lambda_q2"); V[85, 0:64] = f("lambda_k2")
        qg, kg = f("mla_q_g"), f("mla_k_g")
        V[86, 64:80] = qg[80:96]; V[86, 80:96] = qg[64:80]
        V[87, 64:80] = kg[80:96]; V[87, 80:96] = kg[64:80]
        mbv = np.zeros((128, 48), np.float32)
        for i in range(16):
            vis = oth[i] < own[i]
            mbv[:, i] = 0.0 if vis else NEG
            mbv[:, 16 + i] = 0.0 if vis else 1.0
            mbv[:, 32 + i] = 1.0 if vis else 0.0
        in_maps.append({
            "xp": np.ascontiguousarray(xt), "posp": np.ascontiguousarray(pt), "vrows": V, "cst": cst, "mb": mbv,
            "rel_bias": np.ascontiguousarray(np.asarray(inputs["rel_bias"], np.float32).reshape(1, 128)),
            "w_ada": f("w_ada"), "w_in": f("w_in"), "w_uq": f("w_uq"), "w_ukv": f("w_ukv"), "w_o": f("w_o"),
            "w_r": w_r, "b_r": b_r, "w_gate": f("w_gate"), "w_up": f("w_up"), "w_down": f("w_down"),
        })
        metas.append((b, own))
    if "nc" not in _NC_CACHE:
        _NC_CACHE["nc"] = build_program()
    res = run_bass_kernel_spmd(_NC_CACHE["nc"], in_maps, core_ids=list(range(8)))
    outp = np.zeros((4, 32, 128, 1024), np.float32)
    for c in range(8):
        b, own = metas[c]
        o = np.asarray(res.results[c]["out"]).reshape(16, 128, 1024)
        outp[b, own] = o
    return outp.reshape(4, 4096, 1024)
```

```python
import math
from contextlib import ExitStack

import numpy as np
import concourse.bass as bass
import concourse.mybir as mybir
from concourse.bass_utils import run_bass_kernel_spmd

F32 = mybir.dt.float32
BF16 = mybir.dt.bfloat16
I32 = mybir.dt.int32
AF = mybir.ActivationFunctionType
ALU = mybir.AluOpType
AX = mybir.AxisListType

EPS = 1e-6
NEG = -30000.0
LAMBDA_INIT = 0.8 - 0.6 * math.exp(-0.3 * 0)
N_EXP_RUN = 16

ENGS = ("pe", "act", "dve", "pool", "sp")


class Op:
    __slots__ = ("eng", "fn", "deps", "idx", "signal", "count", "dma_key", "dma_count", "is_dma")

    def __init__(self, eng, fn, deps):
        self.eng, self.fn, self.deps = eng, fn, deps
        self.idx, self.signal, self.count = -1, False, 0
        self.is_dma, self.dma_key, self.dma_count = False, None, 0


class Prog:
    def __init__(self):
        self.ops = {e: [] for e in ENGS}
        self.dma_counts = {}
        self.last_w = {}
        self.readers = {}

    def op(self, eng, fn, reads=(), writes=(), dma_key=None):
        ps_reads = [k for k in reads if isinstance(k, str) and k.startswith("ps") and k[2:].isdigit()]
        if ps_reads:
            reads = [k for k in reads if k not in ps_reads]
            writes = list(writes) + [k for k in ps_reads if k not in writes]
        deps = []
        for k in reads:
            w = self.last_w.get(k)
            if w is not None:
                deps.append(w)
        for k in writes:
            w = self.last_w.get(k)
            if w is not None:
                deps.append(w)
            deps.extend(self.readers.get(k, ()))
        o = Op(eng, fn, list(dict.fromkeys(deps)))
        o.idx = len(self.ops[eng])
        self.ops[eng].append(o)
        if dma_key is not None:
            o.is_dma = True
            o.dma_key = dma_key
            self.dma_counts[dma_key] = self.dma_counts.get(dma_key, 0) + 16
            o.dma_count = self.dma_counts[dma_key]
        for k in reads:
            self.readers.setdefault(k, []).append(o)
        for k in writes:
            self.last_w[k] = o
            self.readers[k] = []
        return o

    def emit(self, nc, stack):
        for e in ENGS:
            for o in self.ops[e]:
                latest = {}
                for d in o.deps:
                    if d.is_dma:
                        continue
                    if d.eng == o.eng and o.eng == "pe":
                        continue
                    if d.eng not in latest or d.idx > latest[d.eng].idx:
                        latest[d.eng] = d
                o.deps = [d for d in o.deps if d.is_dma] + list(latest.values())
                for d in latest.values():
                    d.signal = True
        sems = {}
        for e in ENGS:
            sems[e] = stack.enter_context(nc.semaphore("s_" + e))
            c = 0
            for o in self.ops[e]:
                if o.signal:
                    c += 1
                    o.count = c
        for k in self.dma_counts:
            sems["dma_" + k] = stack.enter_context(nc.semaphore("d_" + k))
        block = stack.enter_context(nc.Block())
        engmap = {"pe": "tensor", "act": "scalar", "dve": "vector", "pool": "gpsimd", "sp": "sync"}

        def make(e):
            def body(eng):
                waited = {}
                for o in self.ops[e]:
                    need = {}
                    for d in o.deps:
                        if d.is_dma:
                            sk, val = "dma_" + d.dma_key, d.dma_count
                        else:
                            if d.eng == e and e == "pe":
                                continue
                            sk, val = d.eng, d.count
                        need[sk] = max(need.get(sk, 0), val)
                    for sk, val in need.items():
                        if waited.get(sk, 0) >= val:
                            continue
                        eng.wait_ge(sems[sk], val)
                        waited[sk] = val
                    ins = o.fn(eng)
                    if o.is_dma:
                        ins.then_inc(sems["dma_" + o.dma_key], 16)
                    elif o.signal:
                        ins.then_inc(sems[e], 1)
            return body

        self.stats = {e: (len(self.ops[e]), max([o.count for o in self.ops[e]] + [0])) for e in ENGS}
        for e in ENGS:
            if self.ops[e]:
                getattr(block, engmap[e])(make(e))


def build_program(stop=None):
    nc = bass.Bass("TRN2", target_bir_lowering=False)

    def din(name, shape, dt=F32):
        return nc.dram_tensor(name, shape, dt, kind="ExternalInput").ap()

    xp = din("xp", [4096, 1024])
    posp = din("posp", [1, 4096], I32)
    vrows = din("vrows", [96, 128])
    cst = din("cst", [128, 8])
    mb = din("mb", [128, 48])
    rel_bias = din("rel_bias", [1, 128])
    w_ada = din("w_ada", [1024, 6144])
    w_in = din("w_in", [1024, 2208])
    w_uq = din("w_uq", [384, 768])
    w_ukv = din("w_ukv", [256, 1024])
    w_o = din("w_o", [1024, 1024])
    w_r = din("w_r", [1024, 20])
    b_r = din("b_r", [1, 20])
    w_gate = din("w_gate", [16, 1024, 512])
    w_up = din("w_up", [16, 1024, 512])
    w_down = din("w_down", [16, 512, 1024])
    out = nc.dram_tensor("out", [2048, 1024], F32, kind="ExternalOutput").ap()

    st = ExitStack()
    with st:
        KB = 512
        arena = st.enter_context(nc.sbuf_tensor("arena", [128, 200 * KB], BF16))

        def region(off_kb, size_kb):
            return arena[:, int(off_kb * KB):int((off_kb + size_kb) * KB)]

        U1 = region(0, 64)
        U2 = region(64, 48)
        AT = region(112, 32)
        RA = region(144, 16)
        HB = region(160, 20)
        BC = region(180, 12)
        MI = region(192, 8)

        def sb(name, shape, dt=F32):
            return st.enter_context(nc.sbuf_tensor(name, shape, dt))

        ident_f = sb("ident_f", [128, 128])
        ident_b = sb("ident_b", [128, 128], BF16)
        ones_f = sb("ones_f", [128, 128])
        ones_b = sb("ones_b", [128, 128], BF16)
        blk64 = sb("blk64", [128, 128], BF16)
        V = sb("V", [96, 128])
        col = sb("col", [128, 96])
        cstt = sb("cstt", [128, 8])
        mbt = sb("mbt", [128, 48])
        sil_b = sb("sil_b", [128, 8], BF16)
        modcol = sb("modcol", [128, 48])
        gcol = sb("gcol", [128, 16])
        misc = sb("misc", [128, 16])
        stat = sb("stat", [128, 64])
        stat2 = sb("stat2", [128, 64])
        gate = sb("gate", [128, 16 * 16])
        rt = sb("rt", [128, 64])
        wr_f = sb("wr_f", [128, 8 * 20])
        br_f = sb("br_f", [1, 20])
        relb = sb("relb", [128, 128])
        dgt = sb("dgt", [128, 2 * 128])
        steps = U2[:, 19200:19712].bitcast(F32)
        kpeW = U2[:, 17664:18432]
        kpeWr = U2[:, 18432:19200]

        ps = [st.enter_context(nc.psum_tensor("ps%d" % i, [128, 512], F32)) for i in range(8)]

        p = Prog()

        def finish():
            x1f = U1.bitcast(F32).rearrange("p (s n) -> p s n", s=16)
            outs_ = []
            for s_ in range(16):
                outs_.append(p.op("sp", (lambda s__: (lambda e: e.dma_start(out=out[s__ * 128:(s__ + 1) * 128, :],
                                                                           in_=x1f[:, s__, :])))(s_),
                                  [("x1", s_)] + [("hT", b_) for b_ in range(8)], [], dma_key="out"))
            fin_ = p.op("sp", lambda e: e.nop(), [], [])
            fin_.deps.extend(outs_)
            p.emit(nc, st)
            nc._prog_stats = (p.stats, dict(p.dma_counts))
            return nc

        def mm(out_ap, lhsT, rhs, start, stop, reads, writes):
            return p.op("pe", lambda e: e.matmul(out_ap, lhsT=lhsT, rhs=rhs, start=start, stop=stop,
                                                 skip_group_check=True), reads, writes)

        def tr(out_ap, in_ap, ident, reads, writes):
            return p.op("pe", lambda e: e.transpose(out_ap, in_ap, ident), reads, writes)

        def act(out_ap, in_ap, func, reads, writes, **kw):
            return p.op("act", lambda e: e.activation(out=out_ap, in_=in_ap, func=func, **kw), reads, writes)

        def tt(eng, out_ap, a, b, op, reads, writes):
            return p.op(eng, lambda e: e.tensor_tensor(out=out_ap, in0=a, in1=b, op=op), reads, writes)

        def ts(eng, out_ap, a, s1, s2, op0, op1, reads, writes):
            if op1 is None:
                return p.op(eng, lambda e: e.tensor_scalar(out=out_ap, in0=a, scalar1=s1, scalar2=None, op0=op0),
                            reads, writes)
            return p.op(eng, lambda e: e.tensor_scalar(out=out_ap, in0=a, scalar1=s1, scalar2=s2, op0=op0, op1=op1),
                        reads, writes)

        def stt(out_ap, a, s, b, op0, op1, reads, writes):
            return p.op("dve", lambda e: e.scalar_tensor_tensor(out=out_ap, in0=a, scalar=s, in1=b, op0=op0, op1=op1),
                        reads, writes)

        def cp(eng, out_ap, in_ap, reads, writes):
            if eng == "act":
                return p.op("act", lambda e: e.copy(out=out_ap, in_=in_ap), reads, writes)
            return p.op(eng, lambda e: e.tensor_copy(out=out_ap, in_=in_ap), reads, writes)

        def memset(eng, ap, val, reads, writes):
            return p.op(eng, lambda e: e.memset(ap, val), reads, writes)

        def dma(q, out_ap, in_ap, key, reads, writes):
            return p.op(q, lambda e: e.dma_start(out=out_ap, in_=in_ap), reads, writes, dma_key=key)

        fdummy = sb("fdummy", [128, 8])

        def fence(keys):
            return p.op("dve", lambda e: e.memset(fdummy[:, 0:1], 0.0), [], list(keys) + ["fdummy"])

        def recip(out_ap, in_ap, reads, writes):
            return p.op("dve", lambda e: e.reciprocal(out=out_ap, in_=in_ap), reads, writes)

        def rmax(out_ap, in_ap, reads, writes):
            return p.op("dve", lambda e: e.reduce_max(out=out_ap, in_=in_ap, axis=AX.X), reads, writes)

        epsc = sb("epsc", [128, 1])

        def rsqrt_chain(out_ap, in_ap, inv_n, reads, writes, pr=(0, 128)):
            act(out_ap, in_ap, AF.Ln, list(reads) + ["epsc"], writes, scale=inv_n, bias=epsc[pr[0]:pr[1], :])
            act(out_ap, out_ap, AF.Exp, writes, writes, scale=-0.5)

        memset("pool", ones_f[:], 1.0, [], ["ones_f"])
        memset("pool", epsc[:], EPS, [], ["epsc"])
        p.op("pool", lambda e: e.affine_select(out=ident_f[:], in_=ones_f[:], pattern=[[-1, 128]],
                                               compare_op=ALU.is_equal, fill=0.0, base=0, channel_multiplier=1),
             ["ones_f"], ["ident_f"])
        cp("dve", ident_b[:], ident_f[:], ["ident_f"], ["ident_b"])
        cp("dve", ones_b[:], ones_f[:], ["ones_f"], ["ones_b"])
        memset("dve", blk64[:], 0.0, [], ["blk64"])
        memset("dve", blk64[0:64, 0:64], 1.0, [], ["blk64"])
        memset("dve", blk64[64:128, 64:128], 1.0, [], ["blk64"])
        dma("sp", V[:], vrows[:, :], "V", [], ["V"])
        dma("sp", cstt[:], cst[:, :], "cst", [], ["cst"])
        dma("sp", mbt[:], mb[:, :], "mb", [], ["mb"])
        dma("sp", relb[:], rel_bias[0, :].partition_broadcast(128), "relb", [], ["relb"])
        dma("sp", wr_f[:].rearrange("p (k n) -> p k n", k=8), w_r.rearrange("(k p) n -> p k n", p=128),
            "wr", [], ["wr"])
        dma("sp", br_f[:], b_r[:, :], "br", [], ["br"])
        tr(ps[0][:, 0:96], V[:, :], ident_f[0:96, 0:96], ["V", "ident_f"], ["ps0"])
        cp("act", col[:], ps[0][:, 0:96], ["ps0"], ["col"])
        C_C, C_BADA, C_N1G, C_N2G, C_CQG, C_CKVG = 0, 8, 56, 64, 72, 75
        C_QG, C_KG, C_DQG, C_DKG, C_SUBLN, C_LAM, C_QGR, C_KGR = 77, 78, 79, 80, 81, 82, 86, 87
        act(sil_b[:], col[:, C_C:C_C + 8], AF.Silu, ["col"], ["sil"])

        w_in_v = w_in.rearrange("(k p) n -> p k n", p=128)
        kpeW3 = kpeW.rearrange("p (k n) -> p k n", k=8)
        kpeWr3 = kpeWr.rearrange("p (k n) -> p k n", k=8)
        memset("dve", kpeW, 0.0, [], ["kpeW"])
        memset("dve", kpeWr, 0.0, [], ["kpeWr"])
        dma("pool", kpeW3[:, :, 64:96], w_in_v[:, :, 640:672], "kpeW", [], ["kpeW"])
        dma("pool", kpeWr3[:, :, 64:80], w_in_v[:, :, 656:672], "kpeWr", [], ["kpeWr"])
        dma("pool", kpeWr3[:, :, 80:96], w_in_v[:, :, 640:656], "kpeWr2", [], ["kpeWr"])

        w_ada_v = w_ada.rearrange("(k p) n -> p k n", p=128)
        slab = [U1[:, i * 8192:(i + 1) * 8192].rearrange("p (k n) -> p k n", k=8) for i in range(2)]
        for s in range(6):
            dma("pool", slab[s % 2], w_ada_v[:, :, s * 1024:(s + 1) * 1024], "slab%d" % (s % 2), [],
                [("slab", s % 2)])
            for mloc in range(8):
                m = s * 8 + mloc
                for k in range(8):
                    mm(ps[1][:, m:m + 1], slab[s % 2][:, k, mloc * 128:(mloc + 1) * 128], sil_b[:, k:k + 1],
                       k == 0, k == 7, [("slab", s % 2), "sil"], ["ps1"])
        win = U2[:, 0:8 * 2208].rearrange("p (k n) -> p k n", k=8)
        w_in_v = w_in.rearrange("(k p) n -> p k n", p=128)
        for k in range(8):
            dma("pool", win[:, k, :], w_in_v[:, k, :], "win%d" % k, [], [("win", k)])
        WIN = [("win", k) for k in range(8)]
        tt("dve", modcol[:], ps[1][:, 0:48], col[:, C_BADA:C_BADA + 48], ALU.add, ["ps1", "col"], ["modcol"])
        stt(gcol[:, 0:8], modcol[:, 8:16], 1.0, col[:, C_N1G:C_N1G + 8], ALU.add, ALU.mult, ["modcol", "col"], ["gcol"])
        stt(gcol[:, 8:16], modcol[:, 32:40], 1.0, col[:, C_N2G:C_N2G + 8], ALU.add, ALU.mult, ["modcol", "col"],
            ["gcol"])

        bcslot = [BC[:, i * 2048:(i + 1) * 2048].bitcast(F32) for i in range(3)]
        dg = [dgt[:, 0:128], dgt[:, 128:256]]

        def expand(colsrc, slot, src_keys, pbank):
            for j in range(8):
                ts("dve", dg[j % 2], ident_f[:], colsrc[:, j:j + 1], None, ALU.mult, None,
                   ["ident_f"] + src_keys, [("dg", j % 2)])
                bank = ps[pbank + j // 4]
                mm(bank[:, (j % 4) * 128:(j % 4 + 1) * 128], ones_f[:], dg[j % 2], True, True,
                   [("dg", j % 2), "ones_f"], ["ps%d" % (pbank + j // 4)])
                if j % 4 == 3:
                    cp("act", bcslot[slot][:, (j // 4) * 512:(j // 4 + 1) * 512], bank[:, :],
                       ["ps%d" % (pbank + j // 4)], [("bc", slot)])

        expand(gcol[:, 0:8], 0, ["gcol"], 2)
        expand(modcol[:, 0:8], 1, ["modcol"], 2)

        fence([("slab", 0), ("slab", 1)] + [("hT", b_) for b_ in range(8)])
        if stop == 'p0':
            return finish()
        hT = U1.rearrange("p (b k c) -> p b k c", b=8, k=8)
        xs = [RA[:, i * 2048:(i + 1) * 2048].bitcast(F32) for i in range(2)]
        hb = [RA[:, 4096 + i * 1024:4096 + (i + 1) * 1024] for i in range(2)]
        psT = [ps[4][:].bitcast(BF16), ps[5][:].bitcast(BF16)]
        for t in range(32):
            i = t % 2
            blk, tc_ = t // 4, (t % 4) * 128
            dma("sp", xs[i], xp[t * 128:(t + 1) * 128, :], "xs%d" % i, [], [("xs", i)])
            act(hb[i], xs[i], AF.Square, [("xs", i)], [("hb", i), "stat"], accum_out=stat[:, t:t + 1])
            rsqrt_chain(stat[:, 32 + t:33 + t], stat[:, t:t + 1], 1.0 / 1024, ["stat"], ["stat"])
            stt(xs[i], xs[i], stat[:, 32 + t:33 + t], bcslot[0], ALU.mult, ALU.mult, [("xs", i), "stat", ("bc", 0)],
                [("xs", i)])
            tt("dve", hb[i], xs[i], bcslot[1], ALU.add, [("xs", i), ("bc", 1)], [("hb", i)])
            for k in range(8):
                tr(psT[i][:, k * 128:(k + 1) * 128], hb[i][:, k * 128:(k + 1) * 128], ident_b[:],
                   [("hb", i), "ident_b"], ["ps%d" % (4 + i)])
            cp("act", hT[:, blk, :, tc_:tc_ + 128], psT[i].rearrange("p (k c) -> p k c", k=8), ["ps%d" % (4 + i)],
               [("hT", blk)])

        if stop == 'A':
            return finish()
        C1 = RA[:, 0:4096]
        C2 = RA[:, 4096:8192]
        posi = HB[:, 0:8192].bitcast(I32)
        ang = HB[:, 0:8192].bitcast(F32)
        RA_KEYS = [("xs", 0), ("xs", 1), ("hb", 0), ("hb", 1)]
        fence(RA_KEYS + ["C1", "C2"])
        dma("sp", posi, posp[0, :].partition_broadcast(128), "posi", [], ["HB"])
        TWO_PI = 2.0 * math.pi
        CW1 = 6.28125
        CW2 = TWO_PI - CW1
        nint = bcslot[2].bitcast(I32)
        nflt = bcslot[2]
        for (Ct, shc, key) in ((C1, 1, "C1"), (C2, 2, "C2")):
            for q4 in range(4):
                sl = slice(q4 * 1024, (q4 + 1) * 1024)
                tmpf = MI[:, 0:2048].bitcast(F32)
                K1, K2 = ["mi_tmp"], [("bc", 2)]
                cp("dve", tmpf, posi[:, sl], ["HB"], K1)
                ts("dve", tmpf, tmpf, cstt[:, 0:1], cstt[:, shc:shc + 1], ALU.mult, ALU.add, K1 + ["cst"], K1)
                ts("dve", nflt, tmpf, 1.0 / TWO_PI, None, ALU.mult, None, K1, K2)
                cp("dve", nint, nflt, K2, K2)
                cp("dve", nflt, nint, K2, K2)
                stt(tmpf, nflt, -CW1, tmpf, ALU.mult, ALU.add, K1 + K2, K1)
                stt(tmpf, nflt, -CW2, tmpf, ALU.mult, ALU.add, K1 + K2, K1)
                ts("dve", nflt, tmpf, math.pi, -TWO_PI, ALU.is_gt, ALU.mult, K1, K2)
                tt("dve", tmpf, tmpf, nflt, ALU.add, K1 + K2, K1)
                ts("dve", nflt, tmpf, -math.pi, TWO_PI, ALU.is_lt, ALU.mult, K1, K2)
                tt("dve", tmpf, tmpf, nflt, ALU.add, K1 + K2, K1)
                ts("dve", tmpf, tmpf, math.pi, -math.pi, ALU.min, ALU.max, K1, K1)
                act(Ct[:, sl], tmpf, AF.Sin, K1, [key])

        if stop == 'rope':
            return finish()
        tt("dve", misc[:, 0:1], col[:, C_DQG:C_DQG + 1], col[:, C_DKG:C_DKG + 1], ALU.mult, ["col"], ["misc0"])
        tt("dve", misc[:, 4:5], col[:, C_LAM:C_LAM + 1], col[:, C_LAM + 1:C_LAM + 2], ALU.mult, ["col"], ["misc4"])
        tt("dve", misc[:, 5:6], col[:, C_LAM + 2:C_LAM + 3], col[:, C_LAM + 3:C_LAM + 4], ALU.mult, ["col"], ["misc4"])
        mm(ps[0][:, 0:2], ones_f[:], misc[:, 4:6], True, True, ["misc4", "ones_f"], ["ps0"])
        act(misc[:, 6:8], ps[0][:, 0:2], AF.Exp, ["ps0"], ["misc6"])
        stt(misc[:, 3:4], misc[:, 7:8], -LAMBDA_INIT, misc[:, 6:7], ALU.add, ALU.subtract, ["misc6"], ["misc3"])
        ts("dve", misc[:, 8:9], col[:, C_SUBLN:C_SUBLN + 1], 1.0 - LAMBDA_INIT, None, ALU.mult, None, ["col"],
           ["misc8"])

        Rt = MI[:, 2048:2048 + 512].bitcast(F32)
        p.op("pool", lambda e: e.iota(Rt, [[-1, 256]], base=0, channel_multiplier=1,
                                      allow_small_or_imprecise_dtypes=True), [], ["Rt"])
        biasT = [U2[:, 20480 + h * 512:20480 + (h + 1) * 512].bitcast(F32) for h in range(4)]
        relb3 = relb[:].rearrange("p (b h) -> p b h", h=4)
        neg_thr = [(1, 1), (2, 2), (3, 3), (4, 4), (5, 5), (6, 6), (7, 7), (8, 8), (12, 9), (16, 10), (23, 11),
                   (32, 12), (46, 13), (64, 14), (91, 15)]
        for h in range(4):
            ts("dve", biasT[h], Rt, 0.0, relb3[:, 0, h:h + 1], ALU.mult, ALU.add, ["Rt", "relb"], [("biasT", h)])
        prev_n, prev_p = 0, 0
        for (thr, b) in neg_thr:
            ts("dve", steps, Rt, float(-thr), None, ALU.is_le, None, ["Rt"], ["steps"])
            for h in range(4):
                tt("dve", misc[:, 10:11], relb3[:, b, h:h + 1], relb3[:, prev_n, h:h + 1], ALU.subtract, ["relb"],
                   ["misc10"])
                stt(biasT[h], steps, misc[:, 10:11], biasT[h], ALU.mult, ALU.add, ["steps", "misc10"],
                    [("biasT", h)])
            ts("dve", steps, Rt, float(thr), None, ALU.is_ge, None, ["Rt"], ["steps"])
            for h in range(4):
                tt("dve", misc[:, 10:11], relb3[:, 16 + b, h:h + 1], relb3[:, prev_p, h:h + 1], ALU.subtract,
                   ["relb"], ["misc10"])
                stt(biasT[h], steps, misc[:, 10:11], biasT[h], ALU.mult, ALU.add, ["steps", "misc10"],
                    [("biasT", h)])
            prev_n, prev_p = b, 16 + b
        for h in range(4):
            ts("dve", biasT[h], biasT[h], relb3[:, 15, h:h + 1], None, ALU.subtract, None,
               [("biasT", h), "relb"], [("biasT", h)])
            act(biasT[h], biasT[h], AF.Exp, [("biasT", h)], [("biasT", h)])
        btmps = [MI[:, 2560 + i * 256:2560 + (i + 1) * 256].bitcast(F32) for i in range(2)]

        if stop == 'bias':
            return finish()
        kT = HB[:, 0:4096]
        Vb = HB[:, 4096:8192]
        qT = HB[:, 8192:10240]
        qTb = BC[:, 4096:6144]
        attnT = AT.rearrange("p (c n) -> p c n", c=8)
        pT = [MI[:, 3072 + i * 512:3072 + (i + 1) * 512] for i in range(2)]
        pT2 = [MI[:, 0:512], MI[:, 512:1024]]
        sqb = MI[:, 1024:1536]
        rsf = MI[:, 1536:2560].bitcast(F32)
        tA = RA[:, 0:1]
        PJ_PA, PJ_NB = [0, 1, 4, 5], [2, 7]
        rsfs = [rsf, MI[:, 3072:4096].bitcast(F32)]
        rsfk = [["rsf"], [("pT", 0), ("pT", 1)]]

        def key_tiles(g):
            res = []
            for j in range(0, 4 * g + 4):
                c0 = 128 * max(0, j - 4 * g)
                res.append((16 + j, c0, "oth", j))
                res.append((j, c0, "own", j))
            return res

        PBUF = [pT[0], pT[1], pT2[0], pT2[1]]

        def attention(h, kind):
            diff = kind == "diff"
            nsub = 2 if diff else 1
            sbanks = [0, 1, 2, 3] if diff else [0, 1, 5, 6]
            scale = (64 ** -0.5) if diff else (96 ** -0.5)
            LOOK = 3
            items = []
            for g in range(4):
                tiles = key_tiles(g)
                for ti, (kt, c0, knd, j) in enumerate(tiles):
                    for sub in range(nsub):
                        items.append((g, ti, len(tiles), kt, c0, knd, j, sub))

            def stage1(idx):
                g, ti, nt, kt, c0, knd, j, sub = items[idx]
                slot = idx % 4
                bank, bkey = ps[sbanks[slot]], "ps%d" % sbanks[slot]
                pbuf, pkey = PBUF[slot], ("pT", slot)
                ncol = 512 - c0
                kc = slice(kt * 128, (kt + 1) * 128)
                qcols = slice(g * 512 + c0, (g + 1) * 512)
                in_group = j >= 4 * g
                qsrc = (qT, qTb)[sub] if diff else qT
                need_bias = diff and in_group
                need_bias2 = diff and knd == "own" and 4 * g <= j + 1 < 4 * g + 4
                mm(bank[:, 0:ncol], kT[:, kc], qsrc[:, qcols], True, True, ["kT", "qT"], [bkey])
                if knd == "oth" and in_group:
                    act(pbuf[:, 0:128], bank[:, 0:128], AF.Exp, [bkey, "mb"], [pkey], scale=scale,
                        bias=mbt[:, j:j + 1])
                    if ncol > 128:
                        act(pbuf[:, 128:ncol], bank[:, 128:ncol], AF.Exp, [bkey], [pkey], scale=scale)
                else:
                    act(pbuf[:, 0:ncol], bank[:, 0:ncol], AF.Exp, [bkey], [pkey], scale=scale)
                if need_bias:
                    bsl = biasT[h][:, 0:128] if knd == "own" else biasT[h][:, 128:256]
                    tt("pool", pbuf[:, 0:128], pbuf[:, 0:128], bsl, ALU.mult, [pkey, ("biasT", h)], [pkey])
                if need_bias2:
                    bt = btmps[(j + 1) % 2]
                    if sub == 0:
                        ts("dve", bt, biasT[h][:, 128:256], mbt[:, 16 + j + 1:16 + j + 2],
                           mbt[:, 32 + j + 1:32 + j + 2], ALU.mult, ALU.add,
                           [("biasT", h), "mb"], [("btmp", (j + 1) % 2)])
                    cs = 128 * (j + 1 - 4 * g) - c0
                    tt("pool", pbuf[:, cs:cs + 128], pbuf[:, cs:cs + 128], bt, ALU.mult,
                       [pkey, ("btmp", (j + 1) % 2)], [pkey])
                if knd == "own" and in_group:
                    memset("pool", pbuf[64:128, 0:64], 0.0, [], [pkey])

            def stage2(idx):
                g, ti, nt, kt, c0, knd, j, sub = items[idx]
                slot = idx % 4
                pbuf, pkey = PBUF[slot], ("pT", slot)
                ncol = 512 - c0
                kc = slice(kt * 128, (kt + 1) * 128)
                first, last = ti == 0, ti == nt - 1
                if diff:
                    ob, okey = ps[4 + sub], "ps%d" % (4 + sub)
                    mm(ob[:, c0:512], Vb[:, kc], pbuf[:, 0:ncol], first, last, ["V_h", pkey], [okey])
                    db, dkey = ps[6 + sub], "ps%d" % (6 + sub)
                    mm(db[:, c0:512], ones_b[:], pbuf[:, 0:ncol], first, last, ["ones_b", pkey], [dkey])
                else:
                    ob, okey = ps[2 + g % 2], "ps%d" % (2 + g % 2)
                    mm(ob[:, c0:512], Vb[:, kc], pbuf[:, 0:ncol], first, last, ["V_h", pkey], [okey])
                if last and sub == nsub - 1:
                    finalize(g)

            def finalize(g):
                qs = slice(g * 512, (g + 1) * 512)
                s_a = bcslot[0][:, 0:512]
                s_b = bcslot[0][:, 512:1024]
                s_c = bcslot[1][:, 0:512]
                s_d = bcslot[1][:, 512:1024]
                rhi = BC[:, 2048:2560]
                rlo = BC[:, 2560:3072]
                if diff:
                    cp("dve", s_a, ps[6][:, :], ["ps6"], ["s_a"])
                    cp("dve", s_b, ps[7][:, :], ["ps7"], ["s_b"])
                    cp("dve", s_c, ps[4][:, :], ["ps4"], ["s_c"])
                    cp("dve", s_d, ps[5][:, :], ["ps5"], ["s_d"])
                    recip(s_a, s_a, ["s_a"], ["s_a"])
                    recip(s_b, s_b, ["s_b"], ["s_b"])
                    tt("dve", s_a, s_c, s_a, ALU.mult, ["s_c", "s_a"], ["s_a"])
                    stt(s_b, s_d, misc[:, 3:4], s_b, ALU.mult, ALU.mult, ["s_d", "s_b", "misc3"], ["s_b"])
                    tt("dve", s_a, s_a, s_b, ALU.add, ["s_a", "s_b"], ["s_a"])
                    tt("dve", sqb, s_a, s_a, ALU.mult, ["s_a"], ["sqb"])
                    def fin_b_diff(idx_, s_a=s_a, s_c=s_c, qs=qs, g=g):
                        bn = sbanks[idx_ % 4]
                        mm(ps[bn][:, :], ones_b[:], sqb, True, True, ["sqb", "ones_b"], ["ps%d" % bn])
                        rsqrt_chain(s_c, ps[bn][:, :], 1.0 / 128, ["ps%d" % bn], ["s_c"])
                        stt(attnT[:, 4 + h, qs], s_a, misc[:, 8:9], s_c, ALU.mult, ALU.mult,
                            ["s_a", "s_c", "misc8"], [("at", g)])
                    pending.append([8, fin_b_diff])
                else:
                    ob, okey = ps[2 + g % 2], "ps%d" % (2 + g % 2)
                    r0 = 64 if h % 2 == 0 else 0
                    rhi = BC[:, 2048:2560] if h % 2 == 0 else BC[:, 3072:3584]
                    rlo = BC[:, 2560:3072] if h % 2 == 0 else BC[:, 3584:4096]
                    o0 = 0 if h % 2 == 0 else 64
                    rr, orows = slice(r0, r0 + 1), slice(o0, o0 + 64)
                    recip(s_a[rr, :], ob[rr, :], [okey], ["s_a"])
                    cp("dve", rhi[rr, :], s_a[rr, :], ["s_a"], ["s_c"])
                    tt("dve", s_a[rr, :], s_a[rr, :], rhi[rr, :], ALU.subtract, ["s_a", "s_c"], ["s_a"])
                    cp("dve", rlo[rr, :], s_a[rr, :], ["s_a"], ["s_c"])
                    def fin_b(idx_, rhi=rhi, rlo=rlo, orows=orows, ob=ob, okey=okey, qs=qs, s_b=s_b, g=g):
                        mm(ps[4][:, :], ones_b[:, :], rhi[:, :], True, False, ["s_c", "ones_b"], ["ps4"])
                        mm(ps[4][:, :], ones_b[:, :], rlo[:, :], False, True, ["s_c", "ones_b"], ["ps4"])
                        cp("act", s_b[orows, :], ps[4][orows, :], ["ps4"], ["s_b"])
                        tt("dve", attnT[orows, h // 2, qs], ob[orows, :], s_b[orows, :], ALU.mult, [okey, "s_b"],
                           [("at", g)])
                    pending.append([6, fin_b])

            n = len(items)
            pending = []
            for idx in range(n + LOOK):
                if idx < n:
                    stage1(idx)
                if idx - LOOK >= 0:
                    stage2(idx - LOOK)
                for pe_ in list(pending):
                    pe_[0] -= 1
                    if pe_[0] <= 0:
                        pending.remove(pe_)
                        pe_[1](idx)
            for pe_ in pending:
                pe_[1](n + LOOK - 1)

        HTK = [("hT", b) for b in range(8)]

        fence(["HB", "kT", "qT", "V_h"])
        fence(["mi_tmp", "Rt", "rsf", "sqb"] + [("pT", a_) for a_ in range(4)])
        fence([("bc", 0), ("bc", 1), ("bc", 2), "s_a", "s_b", "s_c", "s_d", "t1", "t2"])
        memset("pool", qT[64:128, :], 0.0, [], ["qT"])
        memset("pool", qTb[0:64, :], 0.0, [], ["qT", ("bc", 2), "t1", "t2"])
        Vd = Vb.rearrange("p (t e) -> p t e", t=32)
        for h in range(4):
            cq0, ck0, cv0 = 672 + h * 128, 1184 + h * 128, 1696 + h * 128
            jobs = [("k", ck0, b_, kT, "kT") for b_ in range(8)] + [("q", cq0, b_, qT, "qT") for b_ in range(4)]

            def b_part1(i):
                side, c0w, blk, dst, dkey = jobs[i]
                pa, pak = ps[PJ_PA[i % 4]], "ps%d" % PJ_PA[i % 4]
                nb, nbk = ps[PJ_NB[i % 2]], "ps%d" % PJ_NB[i % 2]
                for k in range(8):
                    mm(pa[:, :], win[:, k, c0w:c0w + 128], hT[:, blk, k, :], k == 0, k == 7,
                       [("win", k), ("hT", blk)], [pak])

            def b_part1b(i):
                pa, pak = ps[PJ_PA[i % 4]], "ps%d" % PJ_PA[i % 4]
                nb, nbk = ps[PJ_NB[i % 2]], "ps%d" % PJ_NB[i % 2]
                act(sqb, pa[:, :], AF.Square, [pak], ["sqb"])
                mm(nb[:, :], blk64[:], sqb, True, True, ["sqb", "blk64"], [nbk])

            def b_part2(i):
                side, c0w, blk, dst, dkey = jobs[i]
                bs = slice(blk * 512, (blk + 1) * 512)
                pa, pak = ps[PJ_PA[i % 4]], "ps%d" % PJ_PA[i % 4]
                nb, nbk = ps[PJ_NB[i % 2]], "ps%d" % PJ_NB[i % 2]
                rs, rsk = rsfs[i % 2], rsfk[i % 2]
                rsqrt_chain(rs, nb[:, :], 1.0 / 64, [nbk], rsk)
                if side == "k":
                    stt(dst[:, bs], pa[:, :], misc[:, 0:1], rs, ALU.mult, ALU.mult, [pak, "misc0"] + rsk, [dkey])
                else:
                    tt("dve", qT[0:64, bs], pa[0:64, :], rs[0:64, :], ALU.mult, [pak] + rsk, [dkey])
                    tt("dve", qTb[64:128, bs], pa[64:128, :], rs[64:128, :], ALU.mult, [pak] + rsk, [dkey])

            b_part1(0)
            b_part1(1)
            for i in range(len(jobs) + 1):
                if i < len(jobs):
                    b_part1b(i)
                if i + 2 < len(jobs):
                    b_part1(i + 2)
                if i >= 1:
                    b_part2(i - 1)
            for t in range(32):
                blk, tc_ = t // 4, (t % 4) * 128
                vbn = 3 if (t // 4) % 2 == 0 else 6
                for k in range(8):
                    mm(ps[vbn][:, (t % 4) * 128:(t % 4 + 1) * 128], hT[:, blk, k, tc_:tc_ + 128],
                       win[:, k, cv0:cv0 + 128], k == 0, k == 7, [("win", k), ("hT", blk)], ["ps%d" % vbn])
                if t % 4 == 3:
                    cp("act", Vd[:, t - 3:t + 1, :], ps[vbn][:, :].rearrange("p (t e) -> p t e", t=4), ["ps%d" % vbn],
                       ["V_h"])
            attention(h, "diff")

        if stop == 'B':
            return finish()
        U1f = U1
        fence(["qT", ("bc", 2), "t1", "t2"])

        def blkv(blk, off, n):
            return U1f[:, blk * 4096 + off:blk * 4096 + off + n]

        import os as _os
        for blk in range(int(_os.environ.get('KBLK', '8'))):
            own = blk < 4
            banks = {}
            for m in range(2):
                for k in range(8):
                    mm(ps[m][:, :], win[:, k, 384 + m * 128:384 + (m + 1) * 128], hT[:, blk, k, :], k == 0, k == 7,
                       [("win", k), ("hT", blk)], ["ps%d" % m])
            if own:
                for m in range(3):
                    for k in range(8):
                        mm(ps[2 + m][:, :], win[:, k, m * 128:(m + 1) * 128], hT[:, blk, k, :], k == 0, k == 7,
                           [("win", k), ("hT", blk)], ["ps%d" % (2 + m)])
            for k in range(8):
                mm(ps[5][0:96, :], kpeW3[:, k, :], hT[:, blk, k, :], k == 0, k == 7, ["kpeW", ("hT", blk)], ["ps5"])
            for k in range(8):
                mm(ps[6][0:96, :], kpeWr3[:, k, :], hT[:, blk, k, :], k == 0, k == 7, ["kpeWr", ("hT", blk)], ["ps6"])
            if stop == 'Ca':
                return finish()
            hk = ("hT", blk)
            for m in range(2):
                act(sqb, ps[m][:, :], AF.Square, ["ps%d" % m], ["sqb"])
                mm(ps[7][:, :], ones_b[:], sqb, m == 0, m == 1, ["sqb", "ones_b"], ["ps7"])
            rsqrt_chain(rsf, ps[7][:, :], 1.0 / 256, ["ps7"], ["rsf"])
            for m in range(2):
                stt(blkv(blk, m * 512, 512), ps[m][:, :], col[:, C_CKVG + m:C_CKVG + m + 1], rsf, ALU.mult, ALU.mult,
                    ["ps%d" % m, "rsf", "col"], [hk])
            if stop == 'Cb':
                return finish()
            if own:
                for m in range(3):
                    act(sqb, ps[2 + m][:, :], AF.Square, ["ps%d" % (2 + m)], ["sqb"])
                    mm(ps[7][:, :], ones_b[:], sqb, m == 0, m == 2, ["sqb", "ones_b"], ["ps7"])
                rsqrt_chain(rsf, ps[7][:, :], 1.0 / 384, ["ps7"], ["rsf"])
                for m in range(3):
                    stt(blkv(blk, 1024 + m * 512, 512), ps[2 + m][:, :], col[:, C_CQG + m:C_CQG + m + 1], rsf,
                        ALU.mult, ALU.mult, ["ps%d" % (2 + m), "rsf", "col"], [hk])
            if stop == 'Cc':
                return finish()
            _sk = _os.environ.get('KSKIP', '')
            if 'a' not in _sk:
                act(blkv(blk, 3072, 512)[0:96, :], ps[5][0:96, :], AF.Square, ["ps5"], [hk])
                memset("pool", blkv(blk, 3072, 512)[96:128, :], 0.0, [], [hk])
            bsl = slice(blk * 512, (blk + 1) * 512)
            t1 = bcslot[2][:, 0:512]
            t2 = bcslot[2][:, 512:1024]
            if 'b' not in _sk:
                stt(t1[:, :], ps[5][:, :], col[:, C_KG:C_KG + 1], C1[:, bsl], ALU.mult, ALU.mult,
                    ["ps5", "col", "C1"], ["t1"])
            if 'c' not in _sk:
                stt(t2[:, :], ps[6][:, :], col[:, C_KGR:C_KGR + 1], C2[:, bsl], ALU.mult, ALU.mult,
                    ["ps6", "col", "C2"], ["t2"])
            if 'd' not in _sk:
                tt("dve", blkv(blk, 2560, 512)[:, :], t1[:, :], t2[:, :], ALU.add, ["t1", "t2"], [hk])

        if stop == 'Cd' :
            return finish()
        if stop == 'C1':
            return finish()
        wuq = U2[:, 0:2304].rearrange("p (k n) -> p k n", k=3)
        wuqr = U2[:, 2304:4608].rearrange("p (k n) -> p k n", k=3)
        wukv = U2[:, 4608:6656].rearrange("p (k n) -> p k n", k=2)
        w_uq_v = w_uq.rearrange("(k p) n -> p k n", p=128)
        w_uq_v4 = w_uq.rearrange("(k p) (h d) -> p k h d", p=128, d=96)
        wuqr4 = U2[:, 2304:4608].rearrange("p (k h d) -> p k h d", k=3, d=96)
        dma("pool", wuq, w_uq_v, "wuq", [], WIN)
        memset("dve", U2[:, 2304:4608], 0.0, [], WIN)
        for k in range(3):
            dma("pool", wuqr4[:, k, :, 64:80], w_uq_v4[:, k, :, 80:96], "wuqr", [], WIN)
            dma("pool", wuqr4[:, k, :, 80:96], w_uq_v4[:, k, :, 64:80], "wuqr", [], WIN)
        dma("pool", wukv, w_ukv.rearrange("(k p) n -> p k n", p=128), "wukv", [], WIN)
        wo = U2[:, 8192:8192 + 8192].rearrange("p (k n) -> p k n", k=8)
        dma("pool", wo, w_o.rearrange("(k p) n -> p k n", p=128), "wo", [], WIN)

        if stop == 'C':
            return finish()
        Vm = Vb.rearrange("p (t e) -> p t e", t=32)
        memset("pool", kT[96:128, :], 0.0, [], ["kT"])
        sqk = MI[:, 2560:3072]
        memset("pool", sqk, 0.0, [], [("btmp", 0), ("btmp", 1), "sqk"])
        memset("pool", sqb[96:128, :], 0.0, [], ["sqb"])
        memset("pool", BC[:, 2048:4096], 0.0, [], ["s_c", "s_d", ("bc", 1)])
        for h in range(8):
            even = h % 2 == 0
            memset("pool", Vb, 0.0, [], ["V_h"])
            memset("pool", Vm[:, :, 64:65] if even else Vm[:, :, 0:1], 1.0, [], ["V_h"])
            def k_part1(blk):
                hk = ("hT", blk)
                pa, pak = ps[PJ_PA[blk % 4]], "ps%d" % PJ_PA[blk % 4]
                nb, nbk = ps[PJ_NB[blk % 2]], "ps%d" % PJ_NB[blk % 2]
                for m in range(2):
                    mm(pa[:, :], wukv[:, m, h * 128:(h + 1) * 128], blkv(blk, m * 512, 512), m == 0, m == 1,
                       WIN + [hk], [pak])

            def k_part1b(blk):
                hk = ("hT", blk)
                pa, pak = ps[PJ_PA[blk % 4]], "ps%d" % PJ_PA[blk % 4]
                nb, nbk = ps[PJ_NB[blk % 2]], "ps%d" % PJ_NB[blk % 2]
                act(sqk[0:64, :], pa[0:64, :], AF.Square, [pak], ["sqk"])
                mm(nb[:, :], ones_b[:, :], sqk[:, :], True, False, ["sqk", "ones_b"], [nbk])
                mm(nb[:, :], ones_b[:, :], blkv(blk, 3072, 512)[:, :], False, True, [hk, "ones_b"], [nbk])

            def k_part2(blk):
                hk = ("hT", blk)
                bs = slice(blk * 512, (blk + 1) * 512)
                pa, pak = ps[PJ_PA[blk % 4]], "ps%d" % PJ_PA[blk % 4]
                nb, nbk = ps[PJ_NB[blk % 2]], "ps%d" % PJ_NB[blk % 2]
                rs, rsk = rsfs[blk % 2], rsfk[blk % 2]
                rsqrt_chain(rs[0:96, :], nb[0:96, :], 1.0 / 96, [nbk], rsk, pr=(0, 96))
                stt(kT[0:64, bs], pa[0:64, :], col[0:64, C_KG:C_KG + 1], rs[0:64, :], ALU.mult, ALU.mult,
                    [pak, "col"] + rsk, ["kT"])
                tt("dve", kT[64:96, bs], blkv(blk, 2560, 512)[64:96, :], rs[64:96, :], ALU.mult, [hk] + rsk, ["kT"])

            def v_tile(t):
                blk, tc_ = t // 4, (t % 4) * 128
                vbn = 3 if (t // 4) % 2 == 0 else 6
                for m in range(2):
                    mm(ps[vbn][:, (t % 4) * 64:(t % 4 + 1) * 64], blkv(blk, m * 512, 512)[:, tc_:tc_ + 128],
                       wukv[:, m, h * 128 + 64:h * 128 + 128], m == 0, m == 1, WIN + [("hT", blk)], ["ps%d" % vbn])
                if t % 4 == 3:
                    dstv = Vm[:, t - 3:t + 1, 0:64] if even else Vm[:, t - 3:t + 1, 64:128]
                    cp("dve", dstv, ps[vbn][:, 0:256].rearrange("p (t e) -> p t e", t=4), ["ps%d" % vbn], ["V_h"])

            k_part1(0)
            k_part1(1)
            for i in range(9):
                if i < 8:
                    k_part1b(i)
                if i + 2 < 8:
                    k_part1(i + 2)
                if i < 8:
                    for t_ in range(4 * i, 4 * i + 4):
                        v_tile(t_)
                if i >= 1:
                    k_part2(i - 1)
            QB = [(0, 1), (4, 5)]

            def q_part1(blk):
                hk = ("hT", blk)
                nb, nbk = ps[PJ_NB[blk % 2]], "ps%d" % PJ_NB[blk % 2]
                for (bank, wbase) in ((QB[blk % 2][0], 0), (QB[blk % 2][1], 2304)):
                    for m in range(3):
                        w0 = wbase + m * 768 + h * 96
                        mm(ps[bank][:, :], U2[:, w0:w0 + 128], blkv(blk, 1024 + m * 512, 512),
                           m == 0, m == 2, WIN + [hk], ["ps%d" % bank])
                ba = QB[blk % 2][0]
                act(sqb[0:96, :], ps[ba][0:96, :], AF.Square, ["ps%d" % ba], ["sqb"])
                mm(nb[:, :], ones_b[:, :], sqb[:, :], True, True, ["sqb", "ones_b"], [nbk])

            def q_part2(blk):
                bs = slice(blk * 512, (blk + 1) * 512)
                nb, nbk = ps[PJ_NB[blk % 2]], "ps%d" % PJ_NB[blk % 2]
                ba, bb = QB[blk % 2]
                rs, rsk = rsfs[blk % 2], rsfk[blk % 2]
                rsqrt_chain(rs[0:96, :], nb[0:96, :], 1.0 / 96, [nbk], rsk, pr=(0, 96))
                t1 = bcslot[2][:, 0:512]
                t2 = bcslot[2][:, 512:1024]
                stt(t1[0:96, :], ps[ba][0:96, :], col[0:96, C_QG:C_QG + 1], C1[0:96, bs], ALU.mult, ALU.mult,
                    ["ps%d" % ba, "col", "C1"], ["t1"])
                stt(t2[0:96, :], ps[bb][0:96, :], col[0:96, C_QGR:C_QGR + 1], C2[0:96, bs], ALU.mult, ALU.mult,
                    ["ps%d" % bb, "col", "C2"], ["t2"])
                tt("dve", t1[0:96, :], t1[0:96, :], t2[0:96, :], ALU.add, ["t1", "t2"], ["t1"])
                tt("dve", qT[0:96, bs], t1[0:96, :], rs[0:96, :], ALU.mult, ["t1"] + rsk, ["qT"])

            for i in range(5):
                if i < 4:
                    q_part1(i)
                if i >= 1:
                    q_part2(i - 1)
            attention(h, "mla")

        if stop == 'D':
            return finish()
        ATK = [("at", g) for g in range(4)]
        fence([("bc", 0), ("bc", 1), ("bc", 2), "s_a", "s_b", "s_c", "s_d", "t1", "t2"])
        fence(["C1", "C2", "xs2", ("h2f", 0), ("h2f", 1), "h2Tf"])
        expand(modcol[:, 16:24], 0, ["modcol"], 6)
        expand(gcol[:, 8:16], 1, ["gcol"], 6)
        expand(modcol[:, 24:32], 2, ["modcol"], 6)
        x1 = U1.bitcast(F32).rearrange("p (s n) -> p s n", s=16)
        h2T = attnT
        xs2 = RA[:, 0:2048].bitcast(F32)
        h2f = RA[:, 2048:4096].bitcast(F32)
        h2Tf = RA[:, 4096:6144].bitcast(F32).rearrange("p (k c) -> p k c", k=8)
        gate3 = gate[:].rearrange("p (s e) -> p s e", s=16)
        wr3 = wr_f[:].rearrange("p (k n) -> p k n", k=8)
        h2fs = [RA[:, 2048:4096].bitcast(F32), RA[:, 6144:8192].bitcast(F32)]

        def e_part1(s):
            h2f, h2fk = h2fs[s % 2], ("h2f", s % 2)
            sc = slice(s * 128, (s + 1) * 128)
            hk = ("hT", s // 2)
            dma("sp", xs2, xp[s * 128:(s + 1) * 128, :], "xs2", [], ["xs2"])
            wob = (0, 1) if s % 2 == 0 else (5, 6)
            for half in range(2):
                for c in range(8):
                    mm(ps[wob[half]][:, :], attnT[:, c, sc], wo[:, c, half * 512:(half + 1) * 512], c == 0, c == 7,
                       ATK + WIN, ["ps%d" % wob[half]])
            for half in range(2):
                hs = slice(half * 512, (half + 1) * 512)
                tt("dve", x1[:, s, hs], ps[wob[half]][:, :], bcslot[0][:, hs], ALU.mult,
                   ["ps%d" % wob[half], ("bc", 0)], [hk, ("x1", s)])
                tt("dve", x1[:, s, hs], x1[:, s, hs], xs2[:, hs], ALU.add, ["xs2", ("x1", s)], [("x1", s)])
            act(h2f, x1[:, s, :], AF.Square, [("x1", s)], [h2fk, "stat2"], accum_out=stat2[:, s:s + 1])
            rsqrt_chain(stat2[:, 32 + s:33 + s], stat2[:, s:s + 1], 1.0 / 1024, ["stat2"], ["stat2"])
            stt(h2f, x1[:, s, :], stat2[:, 32 + s:33 + s], bcslot[1], ALU.mult, ALU.mult,
                [("x1", s), "stat2", ("bc", 1)], [h2fk])
            tt("dve", h2f, h2f, bcslot[2], ALU.add, [h2fk, ("bc", 2)], [h2fk])

        def e_part2(s):
            h2f, h2fk = h2fs[s % 2], ("h2f", s % 2)
            sc = slice(s * 128, (s + 1) * 128)
            for k in range(8):
                tr(ps[2 + k // 4][:, (k % 4) * 128:(k % 4 + 1) * 128], h2f[:, k * 128:(k + 1) * 128], ident_f[:],
                   [h2fk, "ident_f"], ["ps%d" % (2 + k // 4)])
            for hf in range(2):
                cp("act", h2T[:, hf * 4:(hf + 1) * 4, sc], ps[2 + hf][:, :].rearrange("p (k c) -> p k c", k=4),
                   ["ps%d" % (2 + hf)], ATK + [("h2T", s)])
                cp("dve", h2Tf[:, hf * 4:(hf + 1) * 4, :], ps[2 + hf][:, :].rearrange("p (k c) -> p k c", k=4),
                   ["ps%d" % (2 + hf)], ["h2Tf"])
            for k in range(8):
                mm(ps[4][:, 0:20], h2Tf[:, k, :], wr3[:, k, :], k == 0, False, ["h2Tf", "wr"], ["ps4"])
            mm(ps[4][:, 0:20], ones_f[0:1, :], br_f[0:1, :], False, True, ["ones_f", "br"], ["ps4"])
            R = rt
            cp("dve", R[:, 0:20], ps[4][:, 0:20], ["ps4"], ["rt"])
            RK = ["rt"]
            rmax(R[:, 20:21], R[:, 0:4], RK, RK)
            ts("dve", R[:, 24:28], R[:, 0:4], R[:, 20:21], None, ALU.is_equal, None, RK, RK)
            ts("dve", R[:, 21:22], R[:, 20:21], -1.0, None, ALU.mult, None, RK, RK)
            act(R[:, 28:32], R[:, 0:4], AF.Exp, RK, RK, bias=R[:, 21:22], accum_out=R[:, 22:23])
            recip(R[:, 23:24], R[:, 22:23], RK, RK)
            ts("dve", R[:, 32:36], R[:, 4:8], R[:, 24:25], None, ALU.mult, None, RK, RK)
            for g_ in range(1, 4):
                stt(R[:, 32:36], R[:, 4 + 4 * g_:8 + 4 * g_], R[:, 24 + g_:25 + g_], R[:, 32:36], ALU.mult, ALU.add,
                    RK, RK)
            rmax(R[:, 36:37], R[:, 32:36], RK, RK)
            ts("dve", R[:, 40:44], R[:, 32:36], R[:, 36:37], None, ALU.is_equal, None, RK, RK)
            stt(R[:, 44:48], R[:, 40:44], -1e30, R[:, 32:36], ALU.mult, ALU.add, RK, RK)
            rmax(R[:, 37:38], R[:, 44:48], RK, RK)
            ts("dve", R[:, 48:52], R[:, 44:48], R[:, 37:38], None, ALU.is_equal, None, RK, RK)
            tt("dve", R[:, 38:39], R[:, 37:38], R[:, 36:37], ALU.subtract, RK, RK)
            act(R[:, 39:40], R[:, 38:39], AF.Exp, RK, RK)
            ts("dve", R[:, 52:53], R[:, 39:40], 1.0, None, ALU.add, None, RK, RK)
            recip(R[:, 53:54], R[:, 52:53], RK, RK)
            tt("dve", R[:, 53:54], R[:, 53:54], R[:, 23:24], ALU.mult, RK, RK)
            tt("dve", R[:, 54:55], R[:, 53:54], R[:, 39:40], ALU.mult, RK, RK)
            ts("dve", R[:, 56:60], R[:, 40:44], R[:, 53:54], None, ALU.mult, None, RK, RK)
            stt(R[:, 56:60], R[:, 48:52], R[:, 54:55], R[:, 56:60], ALU.mult, ALU.add, RK, RK)
            for g_ in range(4):
                ts("dve", gate3[:, s, 4 * g_:4 * g_ + 4], R[:, 56:60], R[:, 24 + g_:25 + g_], None, ALU.mult, None,
                   RK, ["gate"])


        e_part1(0)
        for s in range(16):
            if s + 1 < 16:
                e_part1(s + 1)
            e_part2(s)
        if stop == 'E':
            return finish()
        expand(modcol[:, 40:48], 0, ["modcol"], 6)
        fence(WIN + [(("wb", i_), t_) for i_ in range(2) for t_ in "gud"] + [("wb", 0), ("wb", 1)])
        fence(["kT", "qT", "V_h", "HB", "wds"])
        fence(["mi_tmp", "Rt", "rsf", "sqb", "sqk", ("btmp", 0), ("btmp", 1), "sgt"] + [("pT", a_) for a_ in range(4)]
              + [("biasT", h_) for h_ in range(4)] + [("heT", j_) for j_ in range(4)])
        wbuf = [U2[:, i * 12288:(i + 1) * 12288] for i in range(2)]
        wd_stage = HB[:, 0:8192].bitcast(F32).rearrange("p (j n) -> p j n", j=4)
        heT = [MI[:, i * 512:(i + 1) * 512] for i in range(4)]
        sgt = MI[:, 2048:3072].bitcast(F32)
        H2K = [("h2T", s) for s in range(16)]
        for e in range(N_EXP_RUN):
            wb = wbuf[e % 2]
            wg = wb[:, 0:4096].rearrange("p (k n) -> p k n", k=8)
            wu = wb[:, 4096:8192].rearrange("p (k n) -> p k n", k=8)
            wd = wb[:, 8192:12288].rearrange("p (j n) -> p j n", j=4)
            wk = ("wb", e % 2)
            dma("pool", wg, w_gate[e].rearrange("(k p) n -> p k n", p=128), "wg%d" % (e % 2), [], [wk, (wk, "g")])
            dma("pool", wu, w_up[e].rearrange("(k p) n -> p k n", p=128), "wu%d" % (e % 2), [], [(wk, "u")])
            dma("sp", wd_stage, w_down[e].rearrange("(j p) n -> p j n", p=128), "wds", [], ["wds"])
            for j in range(4):
                tt("pool", wd[:, j, :], wd_stage[:, j, :], bcslot[0], ALU.mult, ["wds", ("bc", 0)], [(wk, "d")])
            for blk in range(4):
                bs = slice(blk * 512, (blk + 1) * 512)
                for j in range(4):
                    for k in range(8):
                        mm(ps[0 + j % 2][:, :], wg[:, k, j * 128:(j + 1) * 128], h2T[:, k, bs], k == 0, k == 7,
                           H2K + [wk, (wk, "g")], ["ps%d" % (j % 2)])
                    for k in range(8):
                        mm(ps[2 + j % 2][:, :], wu[:, k, j * 128:(j + 1) * 128], h2T[:, k, bs], k == 0, k == 7,
                           H2K + [(wk, "u")], ["ps%d" % (2 + j % 2)])
                    act(sgt, ps[j % 2][:, :], AF.Silu, ["ps%d" % (j % 2)], ["sgt"])
                    tt("dve", heT[j], sgt, ps[2 + j % 2][:, :], ALU.mult, ["sgt", "ps%d" % (2 + j % 2)], [("heT", j)])
                for tt_ in range(4):
                    s = blk * 4 + tt_
                    for half in range(2):
                        bank = 4 + (tt_ * 2 + half) % 4
                        for j in range(4):
                            mm(ps[bank][:, :], heT[j][:, tt_ * 128:(tt_ + 1) * 128], wd[:, j, half * 512:(half + 1) * 512],
                               j == 0, j == 3, [("heT", j), (wk, "d")], ["ps%d" % bank])
                        hs = slice(half * 512, (half + 1) * 512)
                        stt(x1[:, s, hs], ps[bank][:, :], gate3[:, s, e:e + 1], x1[:, s, hs], ALU.mult, ALU.add,
                            ["ps%d" % bank, "gate", ("x1", s)], [("x1", s)])
        return finish()


def _own_tiles(half):
    return [j for j in range(32) if ((j % 4) in (0, 3)) == (half == 0)]


_NC_CACHE = {}


def kernel(**inputs):
    x = np.ascontiguousarray(inputs["x"], dtype=np.float32)
    pos = np.asarray(inputs["positions"]).astype(np.int32)
    f = lambda k: np.ascontiguousarray(np.asarray(inputs[k], dtype=np.float32)[0])
    inv_freq = (10000.0 ** (-np.arange(0, 16, dtype=np.float32) / 16.0)).astype(np.float32)
    cst = np.zeros((128, 8), np.float32)
    cst[:, 1] = 0.5 * math.pi
    cst[:, 2] = 0.0
    for i in range(16):
        cst[64 + i, 0] = inv_freq[i]
        cst[80 + i, 0] = inv_freq[i]
        cst[64 + i, 2] = math.pi
    w_r = np.ascontiguousarray(np.concatenate([f("w_rg"), f("w_re")], axis=1))
    b_r = np.ascontiguousarray(np.concatenate([f("b_rg"), f("b_re")])[None, :])
    in_maps = []
    metas = []
    for c in range(8):
        b, half = c // 2, c % 2
        own = _own_tiles(half)
        oth = [j for j in range(32) if j not in own]
        order = own + oth
        xt = x[b].reshape(32, 128, 1024)[order].reshape(4096, 1024)
        pt = pos[b].reshape(32, 128)[order].reshape(1, 4096)
        V = np.zeros((96, 128), np.float32)
        V[0:8] = np.asarray(inputs["c"], np.float32)[b].reshape(8, 128)
        V[8:56] = f("b_ada").reshape(48, 128)
        V[56:64] = f("norm1_g").reshape(8, 128)
        V[64:72] = f("norm2_g").reshape(8, 128)
        V[72:75] = f("mla_cq_g").reshape(3, 128)
        V[75:77] = f("mla_ckv_g").reshape(2, 128)
        V[77, 0:96] = f("mla_q_g")
        V[78, 0:96] = f("mla_k_g")
        V[79, 0:64] = f("diff_q_g"); V[79, 64:128] = f("diff_q_g")
        V[80, 0:64] = f("diff_k_g"); V[80, 64:128] = f("diff_k_g")
        V[81, :] = f("diff_subln_g")
        V[82, 0:64] = f("lambda_q1"); V[83, 0:64] = f("lambda_k1")
        V[84, 0:64] = f("lambda_q2"); V[85, 0:64] = f("lambda_k2")
        qg, kg = f("mla_q_g"), f("mla_k_g")
        V[86, 64:80] = qg[80:96]; V[86, 80:96] = qg[64:80]
        V[87, 64:80] = kg[80:96]; V[87, 80:96] = kg[64:80]
        mbv = np.zeros((128, 48), np.float32)
        for i in range(16):
            vis = oth[i] < own[i]
            mbv[:, i] = 0.0 if vis else NEG
            mbv[:, 16 + i] = 0.0 if vis else 1.0
            mbv[:, 32 + i] = 1.0 if vis else 0.0
        in_maps.append({
            "xp": np.ascontiguousarray(xt), "posp": np.ascontiguousarray(pt), "vrows": V, "cst": cst, "mb": mbv,
            "rel_bias": np.ascontiguousarray(np.asarray(inputs["rel_bias"], np.float32).reshape(1, 128)),
            "w_ada": f("w_ada"), "w_in": f("w_in"), "w_uq": f("w_uq"), "w_ukv": f("w_ukv"), "w_o": f("w_o"),
            "w_r": w_r, "b_r": b_r, "w_gate": f("w_gate"), "w_up": f("w_up"), "w_down": f("w_down"),
        })
        metas.append((b, own))
    if "nc" not in _NC_CACHE:
        _NC_CACHE["nc"] = build_program()
    res = run_bass_kernel_spmd(_NC_CACHE["nc"], in_maps, core_ids=list(range(8)))
    outp = np.zeros((4, 32, 128, 1024), np.float32)
    for c in range(8):
        b, own = metas[c]
        o = np.asarray(res.results[c]["out"]).reshape(16, 128, 1024)
        outp[b, own] = o
    return outp.reshape(4, 4096, 1024)
```

```python
import math
from contextlib import ExitStack

import numpy as np
import concourse.bass as bass
import concourse.mybir as mybir
from concourse.bass_utils import run_bass_kernel_spmd

F32 = mybir.dt.float32
BF16 = mybir.dt.bfloat16
I32 = mybir.dt.int32
AF = mybir.ActivationFunctionType
ALU = mybir.AluOpType
AX = mybir.AxisListType

EPS = 1e-6
NEG = -30000.0
LAMBDA_INIT = 0.8 - 0.6 * math.exp(-0.3 * 0)
N_EXP_RUN = 16

ENGS = ("pe", "act", "dve", "pool", "sp")


class Op:
    __slots__ = ("eng", "fn", "deps", "idx", "signal", "count", "dma_key", "dma_count", "is_dma")

    def __init__(self, eng, fn, deps):
        self.eng, self.fn, self.deps = eng, fn, deps
        self.idx, self.signal, self.count = -1, False, 0
        self.is_dma, self.dma_key, self.dma_count = False, None, 0


class Prog:
    def __init__(self):
        self.ops = {e: [] for e in ENGS}
        self.dma_counts = {}
        self.last_w = {}
        self.readers = {}

    def op(self, eng, fn, reads=(), writes=(), dma_key=None):
        ps_reads = [k for k in reads if isinstance(k, str) and k.startswith("ps") and k[2:].isdigit()]
        if ps_reads:
            reads = [k for k in reads if k not in ps_reads]
            writes = list(writes) + [k for k in ps_reads if k not in writes]
        deps = []
        for k in reads:
            w = self.last_w.get(k)
            if w is not None:
                deps.append(w)
        for k in writes:
            w = self.last_w.get(k)
            if w is not None:
                deps.append(w)
            deps.extend(self.readers.get(k, ()))
        o = Op(eng, fn, list(dict.fromkeys(deps)))
        o.idx = len(self.ops[eng])
        self.ops[eng].append(o)
        if dma_key is not None:
            o.is_dma = True
            o.dma_key = dma_key
            self.dma_counts[dma_key] = self.dma_counts.get(dma_key, 0) + 16
            o.dma_count = self.dma_counts[dma_key]
        for k in reads:
            self.readers.setdefault(k, []).append(o)
        for k in writes:
            self.last_w[k] = o
            self.readers[k] = []
        return o

    def emit(self, nc, stack):
        for e in ENGS:
            for o in self.ops[e]:
                latest = {}
                for d in o.deps:
                    if d.is_dma:
                        continue
                    if d.eng == o.eng and o.eng == "pe":
                        continue
                    if d.eng not in latest or d.idx > latest[d.eng].idx:
                        latest[d.eng] = d
                o.deps = [d for d in o.deps if d.is_dma] + list(latest.values())
                for d in latest.values():
                    d.signal = True
        sems = {}
        for e in ENGS:
            sems[e] = stack.enter_context(nc.semaphore("s_" + e))
            c = 0
            for o in self.ops[e]:
                if o.signal:
                    c += 1
                    o.count = c
        for k in self.dma_counts:
            sems["dma_" + k] = stack.enter_context(nc.semaphore("d_" + k))
        block = stack.enter_context(nc.Block())
        engmap = {"pe": "tensor", "act": "scalar", "dve": "vector", "pool": "gpsimd", "sp": "sync"}

        def make(e):
            def body(eng):
                waited = {}
                for o in self.ops[e]:
                    need = {}
                    for d in o.deps:
                        if d.is_dma:
                            sk, val = "dma_" + d.dma_key, d.dma_count
                        else:
                            if d.eng == e and e == "pe":
                                continue
                            sk, val = d.eng, d.count
                        need[sk] = max(need.get(sk, 0), val)
                    for sk, val in need.items():
                        if waited.get(sk, 0) >= val:
                            continue
                        eng.wait_ge(sems[sk], val)
                        waited[sk] = val
                    ins = o.fn(eng)
                    if o.is_dma:
                        ins.then_inc(sems["dma_" + o.dma_key], 16)
                    elif o.signal:
                        ins.then_inc(sems[e], 1)
            return body

        self.stats = {e: (len(self.ops[e]), max([o.count for o in self.ops[e]] + [0])) for e in ENGS}
        for e in ENGS:
            if self.ops[e]:
                getattr(block, engmap[e])(make(e))


def build_program(stop=None):
    nc = bass.Bass("TRN2", target_bir_lowering=False)

    def din(name, shape, dt=F32):
        return nc.dram_tensor(name, shape, dt, kind="ExternalInput").ap()

    xp = din("xp", [4096, 1024])
    posp = din("posp", [1, 4096], I32)
    vrows = din("vrows", [96, 128])
    cst = din("cst", [128, 8])
    mb = din("mb", [128, 48])
    rel_bias = din("rel_bias", [1, 128])
    w_ada = din("w_ada", [1024, 6144])
    w_in = din("w_in", [1024, 2208])
    w_uq = din("w_uq", [384, 768])
    w_ukv = din("w_ukv", [256, 1024])
    w_o = din("w_o", [1024, 1024])
    w_r = din("w_r", [1024, 20])
    b_r = din("b_r", [1, 20])
    w_gate = din("w_gate", [16, 1024, 512])
    w_up = din("w_up", [16, 1024, 512])
    w_down = din("w_down", [16, 512, 1024])
    out = nc.dram_tensor("out", [2048, 1024], F32, kind="ExternalOutput").ap()

    st = ExitStack()
    with st:
        KB = 512
        arena = st.enter_context(nc.sbuf_tensor("arena", [128, 200 * KB], BF16))

        def region(off_kb, size_kb):
            return arena[:, int(off_kb * KB):int((off_kb + size_kb) * KB)]

        U1 = region(0, 64)
        U2 = region(64, 48)
        AT = region(112, 32)
        RA = region(144, 16)
        HB = region(160, 20)
        BC = region(180, 12)
        MI = region(192, 8)

        def sb(name, shape, dt=F32):
            return st.enter_context(nc.sbuf_tensor(name, shape, dt))

        ident_f = sb("ident_f", [128, 128])
        ident_b = sb("ident_b", [128, 128], BF16)
        ones_f = sb("ones_f", [128, 128])
        ones_b = sb("ones_b", [128, 128], BF16)
        blk64 = sb("blk64", [128, 128], BF16)
        V = sb("V", [96, 128])
        col = sb("col", [128, 96])
        cstt = sb("cstt", [128, 8])
        mbt = sb("mbt", [128, 48])
        sil_b = sb("sil_b", [128, 8], BF16)
        modcol = sb("modcol", [128, 48])
        gcol = sb("gcol", [128, 16])
        misc = sb("misc", [128, 16])
        stat = sb("stat", [128, 64])
        stat2 = sb("stat2", [128, 64])
        gate = sb("gate", [128, 16 * 16])
        rt = sb("rt", [128, 64])
        wr_f = sb("wr_f", [128, 8 * 20])
        br_f = sb("br_f", [1, 20])
        relb = sb("relb", [128, 128])
        dgt = sb("dgt", [128, 2 * 128])
        steps = U2[:, 19200:19712].bitcast(F32)
        kpeW = U2[:, 17664:18432]
        kpeWr = U2[:, 18432:19200]

        ps = [st.enter_context(nc.psum_tensor("ps%d" % i, [128, 512], F32)) for i in range(8)]

        p = Prog()

        def finish():
            x1f = U1.bitcast(F32).rearrange("p (s n) -> p s n", s=16)
            outs_ = []
            for s_ in range(16):
                outs_.append(p.op("sp", (lambda s__: (lambda e: e.dma_start(out=out[s__ * 128:(s__ + 1) * 128, :],
                                                                           in_=x1f[:, s__, :])))(s_),
                                  [("x1", s_)] + [("hT", b_) for b_ in range(8)], [], dma_key="out"))
            fin_ = p.op("sp", lambda e: e.nop(), [], [])
            fin_.deps.extend(outs_)
            p.emit(nc, st)
            nc._prog_stats = (p.stats, dict(p.dma_counts))
            return nc

        def mm(out_ap, lhsT, rhs, start, stop, reads, writes):
            return p.op("pe", lambda e: e.matmul(out_ap, lhsT=lhsT, rhs=rhs, start=start, stop=stop,
                                                 skip_group_check=True), reads, writes)

        def tr(out_ap, in_ap, ident, reads, writes):
            return p.op("pe", lambda e: e.transpose(out_ap, in_ap, ident), reads, writes)

        def act(out_ap, in_ap, func, reads, writes, **kw):
            return p.op("act", lambda e: e.activation(out=out_ap, in_=in_ap, func=func, **kw), reads, writes)

        def tt(eng, out_ap, a, b, op, reads, writes):
            return p.op(eng, lambda e: e.tensor_tensor(out=out_ap, in0=a, in1=b, op=op), reads, writes)

        def ts(eng, out_ap, a, s1, s2, op0, op1, reads, writes):
            if op1 is None:
                return p.op(eng, lambda e: e.tensor_scalar(out=out_ap, in0=a, scalar1=s1, scalar2=None, op0=op0),
                            reads, writes)
            return p.op(eng, lambda e: e.tensor_scalar(out=out_ap, in0=a, scalar1=s1, scalar2=s2, op0=op0, op1=op1),
                        reads, writes)

        def stt(out_ap, a, s, b, op0, op1, reads, writes):
            return p.op("dve", lambda e: e.scalar_tensor_tensor(out=out_ap, in0=a, scalar=s, in1=b, op0=op0, op1=op1),
                        reads, writes)

        def cp(eng, out_ap, in_ap, reads, writes):
            if eng == "act":
                return p.op("act", lambda e: e.copy(out=out_ap, in_=in_ap), reads, writes)
            return p.op(eng, lambda e: e.tensor_copy(out=out_ap, in_=in_ap), reads, writes)

        def memset(eng, ap, val, reads, writes):
            return p.op(eng, lambda e: e.memset(ap, val), reads, writes)

        def dma(q, out_ap, in_ap, key, reads, writes):
            return p.op(q, lambda e: e.dma_start(out=out_ap, in_=in_ap), reads, writes, dma_key=key)

        fdummy = sb("fdummy", [128, 8])

        def fence(keys):
            return p.op("dve", lambda e: e.memset(fdummy[:, 0:1], 0.0), [], list(keys) + ["fdummy"])

        def recip(out_ap, in_ap, reads, writes):
            return p.op("dve", lambda e: e.reciprocal(out=out_ap, in_=in_ap), reads, writes)

        def rmax(out_ap, in_ap, reads, writes):
            return p.op("dve", lambda e: e.reduce_max(out=out_ap, in_=in_ap, axis=AX.X), reads, writes)

        epsc = sb("epsc", [128, 1])

        def rsqrt_chain(out_ap, in_ap, inv_n, reads, writes, pr=(0, 128)):
            act(out_ap, in_ap, AF.Ln, list(reads) + ["epsc"], writes, scale=inv_n, bias=epsc[pr[0]:pr[1], :])
            act(out_ap, out_ap, AF.Exp, writes, writes, scale=-0.5)

        memset("pool", ones_f[:], 1.0, [], ["ones_f"])
        memset("pool", epsc[:], EPS, [], ["epsc"])
        p.op("pool", lambda e: e.affine_select(out=ident_f[:], in_=ones_f[:], pattern=[[-1, 128]],
                                               compare_op=ALU.is_equal, fill=0.0, base=0, channel_multiplier=1),
             ["ones_f"], ["ident_f"])
        cp("dve", ident_b[:], ident_f[:], ["ident_f"], ["ident_b"])
        cp("dve", ones_b[:], ones_f[:], ["ones_f"], ["ones_b"])
        memset("dve", blk64[:], 0.0, [], ["blk64"])
        memset("dve", blk64[0:64, 0:64], 1.0, [], ["blk64"])
        memset("dve", blk64[64:128, 64:128], 1.0, [], ["blk64"])
        dma("sp", V[:], vrows[:, :], "V", [], ["V"])
        dma("sp", cstt[:], cst[:, :], "cst", [], ["cst"])
        dma("sp", mbt[:], mb[:, :], "mb", [], ["mb"])
        dma("sp", relb[:], rel_bias[0, :].partition_broadcast(128), "relb", [], ["relb"])
        dma("sp", wr_f[:].rearrange("p (k n) -> p k n", k=8), w_r.rearrange("(k p) n -> p k n", p=128),
            "wr", [], ["wr"])
        dma("sp", br_f[:], b_r[:, :], "br", [], ["br"])
        tr(ps[0][:, 0:96], V[:, :], ident_f[0:96, 0:96], ["V", "ident_f"], ["ps0"])
        cp("act", col[:], ps[0][:, 0:96], ["ps0"], ["col"])
        C_C, C_BADA, C_N1G, C_N2G, C_CQG, C_CKVG = 0, 8, 56, 64, 72, 75
        C_QG, C_KG, C_DQG, C_DKG, C_SUBLN, C_LAM, C_QGR, C_KGR = 77, 78, 79, 80, 81, 82, 86, 87
        act(sil_b[:], col[:, C_C:C_C + 8], AF.Silu, ["col"], ["sil"])

        w_in_v = w_in.rearrange("(k p) n -> p k n", p=128)
        kpeW3 = kpeW.rearrange("p (k n) -> p k n", k=8)
        kpeWr3 = kpeWr.rearrange("p (k n) -> p k n", k=8)
        memset("dve", kpeW, 0.0, [], ["kpeW"])
        memset("dve", kpeWr, 0.0, [], ["kpeWr"])
        dma("pool", kpeW3[:, :, 64:96], w_in_v[:, :, 640:672], "kpeW", [], ["kpeW"])
        dma("pool", kpeWr3[:, :, 64:80], w_in_v[:, :, 656:672], "kpeWr", [], ["kpeWr"])
        dma("pool", kpeWr3[:, :, 80:96], w_in_v[:, :, 640:656], "kpeWr2", [], ["kpeWr"])

        w_ada_v = w_ada.rearrange("(k p) n -> p k n", p=128)
        slab = [U1[:, i * 8192:(i + 1) * 8192].rearrange("p (k n) -> p k n", k=8) for i in range(2)]
        for s in range(6):
            dma("pool", slab[s % 2], w_ada_v[:, :, s * 1024:(s + 1) * 1024], "slab%d" % (s % 2), [],
                [("slab", s % 2)])
            for mloc in range(8):
                m = s * 8 + mloc
                for k in range(8):
                    mm(ps[1][:, m:m + 1], slab[s % 2][:, k, mloc * 128:(mloc + 1) * 128], sil_b[:, k:k + 1],
                       k == 0, k == 7, [("slab", s % 2), "sil"], ["ps1"])
        win = U2[:, 0:8 * 2208].rearrange("p (k n) -> p k n", k=8)
        w_in_v = w_in.rearrange("(k p) n -> p k n", p=128)
        for k in range(8):
            dma("pool", win[:, k, :], w_in_v[:, k, :], "win%d" % k, [], [("win", k)])
        WIN = [("win", k) for k in range(8)]
        tt("dve", modcol[:], ps[1][:, 0:48], col[:, C_BADA:C_BADA + 48], ALU.add, ["ps1", "col"], ["modcol"])
        stt(gcol[:, 0:8], modcol[:, 8:16], 1.0, col[:, C_N1G:C_N1G + 8], ALU.add, ALU.mult, ["modcol", "col"], ["gcol"])
        stt(gcol[:, 8:16], modcol[:, 32:40], 1.0, col[:, C_N2G:C_N2G + 8], ALU.add, ALU.mult, ["modcol", "col"],
            ["gcol"])

        bcslot = [BC[:, i * 2048:(i + 1) * 2048].bitcast(F32) for i in range(3)]
        dg = [dgt[:, 0:128], dgt[:, 128:256]]

        def expand(colsrc, slot, src_keys, pbank):
            for j in range(8):
                ts("dve", dg[j % 2], ident_f[:], colsrc[:, j:j + 1], None, ALU.mult, None,
                   ["ident_f"] + src_keys, [("dg", j % 2)])
                bank = ps[pbank + j // 4]
                mm(bank[:, (j % 4) * 128:(j % 4 + 1) * 128], ones_f[:], dg[j % 2], True, True,
                   [("dg", j % 2), "ones_f"], ["ps%d" % (pbank + j // 4)])
                if j % 4 == 3:
                    cp("act", bcslot[slot][:, (j // 4) * 512:(j // 4 + 1) * 512], bank[:, :],
                       ["ps%d" % (pbank + j // 4)], [("bc", slot)])

        expand(gcol[:, 0:8], 0, ["gcol"], 2)
        expand(modcol[:, 0:8], 1, ["modcol"], 2)

        fence([("slab", 0), ("slab", 1)] + [("hT", b_) for b_ in range(8)])
        if stop == 'p0':
            return finish()
        hT = U1.rearrange("p (b k c) -> p b k c", b=8, k=8)
        xs = [RA[:, i * 2048:(i + 1) * 2048].bitcast(F32) for i in range(2)]
        hb = [RA[:, 4096 + i * 1024:4096 + (i + 1) * 1024] for i in range(2)]
        psT = [ps[4][:].bitcast(BF16), ps[5][:].bitcast(BF16)]
        for t in range(32):
            i = t % 2
            blk, tc_ = t // 4, (t % 4) * 128
            dma("sp", xs[i], xp[t * 128:(t + 1) * 128, :], "xs%d" % i, [], [("xs", i)])
            act(hb[i], xs[i], AF.Square, [("xs", i)], [("hb", i), "stat"], accum_out=stat[:, t:t + 1])
            rsqrt_chain(stat[:, 32 + t:33 + t], stat[:, t:t + 1], 1.0 / 1024, ["stat"], ["stat"])
            stt(xs[i], xs[i], stat[:, 32 + t:33 + t], bcslot[0], ALU.mult, ALU.mult, [("xs", i), "stat", ("bc", 0)],
                [("xs", i)])
            tt("dve", hb[i], xs[i], bcslot[1], ALU.add, [("xs", i), ("bc", 1)], [("hb", i)])
            for k in range(8):
                tr(psT[i][:, k * 128:(k + 1) * 128], hb[i][:, k * 128:(k + 1) * 128], ident_b[:],
                   [("hb", i), "ident_b"], ["ps%d" % (4 + i)])
            cp("act", hT[:, blk, :, tc_:tc_ + 128], psT[i].rearrange("p (k c) -> p k c", k=8), ["ps%d" % (4 + i)],
               [("hT", blk)])

        if stop == 'A':
            return finish()
        C1 = RA[:, 0:4096]
        C2 = RA[:, 4096:8192]
        posi = HB[:, 0:8192].bitcast(I32)
        ang = HB[:, 0:8192].bitcast(F32)
        RA_KEYS = [("xs", 0), ("xs", 1), ("hb", 0), ("hb", 1)]
        fence(RA_KEYS + ["C1", "C2"])
        dma("sp", posi, posp[0, :].partition_broadcast(128), "posi", [], ["HB"])
        TWO_PI = 2.0 * math.pi
        CW1 = 6.28125
        CW2 = TWO_PI - CW1
        nint = bcslot[2].bitcast(I32)
        nflt = bcslot[2]
        for (Ct, shc, key) in ((C1, 1, "C1"), (C2, 2, "C2")):
            for q4 in range(4):
                sl = slice(q4 * 1024, (q4 + 1) * 1024)
                tmpf = MI[:, 0:2048].bitcast(F32)
                K1, K2 = ["mi_tmp"], [("bc", 2)]
                cp("dve", tmpf, posi[:, sl], ["HB"], K1)
                ts("dve", tmpf, tmpf, cstt[:, 0:1], cstt[:, shc:shc + 1], ALU.mult, ALU.add, K1 + ["cst"], K1)
                ts("dve", nflt, tmpf, 1.0 / TWO_PI, None, ALU.mult, None, K1, K2)
                cp("dve", nint, nflt, K2, K2)
                cp("dve", nflt, nint, K2, K2)
                stt(tmpf, nflt, -CW1, tmpf, ALU.mult, ALU.add, K1 + K2, K1)
                stt(tmpf, nflt, -CW2, tmpf, ALU.mult, ALU.add, K1 + K2, K1)
                ts("dve", nflt, tmpf, math.pi, -TWO_PI, ALU.is_gt, ALU.mult, K1, K2)
                tt("dve", tmpf, tmpf, nflt, ALU.add, K1 + K2, K1)
                ts("dve", nflt, tmpf, -math.pi, TWO_PI, ALU.is_lt, ALU.mult, K1, K2)
                tt("dve", tmpf, tmpf, nflt, ALU.add, K1 + K2, K1)
                ts("dve", tmpf, tmpf, math.pi, -math.pi, ALU.min, ALU.max, K1, K1)
                act(Ct[:, sl], tmpf, AF.Sin, K1, [key])

        if stop == 'rope':
            return finish()
        tt("dve", misc[:, 0:1], col[:, C_DQG:C_DQG + 1], col[:, C_DKG:C_DKG + 1], ALU.mult, ["col"], ["misc0"])
        tt("dve", misc[:, 4:5], col[:, C_LAM:C_LAM + 1], col[:, C_LAM + 1:C_LAM + 2], ALU.mult, ["col"], ["misc4"])
        tt("dve", misc[:, 5:6], col[:, C_LAM + 2:C_LAM + 3], col[:, C_LAM + 3:C_LAM + 4], ALU.mult, ["col"], ["misc4"])
        mm(ps[0][:, 0:2], ones_f[:], misc[:, 4:6], True, True, ["misc4", "ones_f"], ["ps0"])
        act(misc[:, 6:8], ps[0][:, 0:2], AF.Exp, ["ps0"], ["misc6"])
        stt(misc[:, 3:4], misc[:, 7:8], -LAMBDA_INIT, misc[:, 6:7], ALU.add, ALU.subtract, ["misc6"], ["misc3"])
        ts("dve", misc[:, 8:9], col[:, C_SUBLN:C_SUBLN + 1], 1.0 - LAMBDA_INIT, None, ALU.mult, None, ["col"],
           ["misc8"])

        Rt = MI[:, 2048:2048 + 512].bitcast(F32)
        p.op("pool", lambda e: e.iota(Rt, [[-1, 256]], base=0, channel_multiplier=1,
                                      allow_small_or_imprecise_dtypes=True), [], ["Rt"])
        biasT = [U2[:, 20480 + h * 512:20480 + (h + 1) * 512].bitcast(F32) for h in range(4)]
        relb3 = relb[:].rearrange("p (b h) -> p b h", h=4)
        neg_thr = [(1, 1), (2, 2), (3, 3), (4, 4), (5, 5), (6, 6), (7, 7), (8, 8), (12, 9), (16, 10), (23, 11),
                   (32, 12), (46, 13), (64, 14), (91, 15)]
        for h in range(4):
            ts("dve", biasT[h], Rt, 0.0, relb3[:, 0, h:h + 1], ALU.mult, ALU.add, ["Rt", "relb"], [("biasT", h)])
        prev_n, prev_p = 0, 0
        for (thr, b) in neg_thr:
            ts("dve", steps, Rt, float(-thr), None, ALU.is_le, None, ["Rt"], ["steps"])
            for h in range(4):
                tt("dve", misc[:, 10:11], relb3[:, b, h:h + 1], relb3[:, prev_n, h:h + 1], ALU.subtract, ["relb"],
                   ["misc10"])
                stt(biasT[h], steps, misc[:, 10:11], biasT[h], ALU.mult, ALU.add, ["steps", "misc10"],
                    [("biasT", h)])
            ts("dve", steps, Rt, float(thr), None, ALU.is_ge, None, ["Rt"], ["steps"])
            for h in range(4):
                tt("dve", misc[:, 10:11], relb3[:, 16 + b, h:h + 1], relb3[:, prev_p, h:h + 1], ALU.subtract,
                   ["relb"], ["misc10"])
                stt(biasT[h], steps, misc[:, 10:11], biasT[h], ALU.mult, ALU.add, ["steps", "misc10"],
                    [("biasT", h)])
            prev_n, prev_p = b, 16 + b
        for h in range(4):
            ts("dve", biasT[h], biasT[h], relb3[:, 15, h:h + 1], None, ALU.subtract, None,
               [("biasT", h), "relb"], [("biasT", h)])
            act(biasT[h], biasT[h], AF.Exp, [("biasT", h)], [("biasT", h)])
        btmps = [MI[:, 2560 + i * 256:2560 + (i + 1) * 256].bitcast(F32) for i in range(2)]

        if stop == 'bias':
            return finish()
        kT = HB[:, 0:4096]
        Vb = HB[:, 4096:8192]
        qT = HB[:, 8192:10240]
        qTb = BC[:, 4096:6144]
        attnT = AT.rearrange("p (c n) -> p c n", c=8)
        pT = [MI[:, 3072 + i * 512:3072 + (i + 1) * 512] for i in range(2)]
        pT2 = [MI[:, 0:512], MI[:, 512:1024]]
        sqb = MI[:, 1024:1536]
        rsf = MI[:, 1536:2560].bitcast(F32)
        tA = RA[:, 0:1]
        PJ_PA, PJ_NB = [0, 1, 4, 5], [2, 7]
        rsfs = [rsf, MI[:, 3072:4096].bitcast(F32)]
        rsfk = [["rsf"], [("pT", 0), ("pT", 1)]]

        def key_tiles(g):
            res = []
            for j in range(0, 4 * g + 4):
                c0 = 128 * max(0, j - 4 * g)
                res.append((16 + j, c0, "oth", j))
                res.append((j, c0, "own", j))
            return res

        PBUF = [pT[0], pT[1], pT2[0], pT2[1]]

        def attention(h, kind):
            diff = kind == "diff"
            nsub = 2 if diff else 1
            sbanks = [0, 1, 2, 3] if diff else [0, 1, 5, 6]
            scale = (64 ** -0.5) if diff else (96 ** -0.5)
            LOOK = 3
            items = []
            for g in range(4):
                tiles = key_tiles(g)
                for ti, (kt, c0, knd, j) in enumerate(tiles):
                    for sub in range(nsub):
                        items.append((g, ti, len(tiles), kt, c0, knd, j, sub))

            def stage1(idx):
                g, ti, nt, kt, c0, knd, j, sub = items[idx]
                slot = idx % 4
                bank, bkey = ps[sbanks[slot]], "ps%d" % sbanks[slot]
                pbuf, pkey = PBUF[slot], ("pT", slot)
                ncol = 512 - c0
                kc = slice(kt * 128, (kt + 1) * 128)
                qcols = slice(g * 512 + c0, (g + 1) * 512)
                in_group = j >= 4 * g
                qsrc = (qT, qTb)[sub] if diff else qT
                need_bias = diff and in_group
                need_bias2 = diff and knd == "own" and 4 * g <= j + 1 < 4 * g + 4
                mm(bank[:, 0:ncol], kT[:, kc], qsrc[:, qcols], True, True, ["kT", "qT"], [bkey])
                if knd == "oth" and in_group:
                    act(pbuf[:, 0:128], bank[:, 0:128], AF.Exp, [bkey, "mb"], [pkey], scale=scale,
                        bias=mbt[:, j:j + 1])
                    if ncol > 128:
                        act(pbuf[:, 128:ncol], bank[:, 128:ncol], AF.Exp, [bkey], [pkey], scale=scale)
                else:
                    act(pbuf[:, 0:ncol], bank[:, 0:ncol], AF.Exp, [bkey], [pkey], scale=scale)
                if need_bias:
                    bsl = biasT[h][:, 0:128] if knd == "own" else biasT[h][:, 128:256]
                    tt("pool", pbuf[:, 0:128], pbuf[:, 0:128], bsl, ALU.mult, [pkey, ("biasT", h)], [pkey])
                if need_bias2:
                    bt = btmps[(j + 1) % 2]
                    if sub == 0:
                        ts("dve", bt, biasT[h][:, 128:256], mbt[:, 16 + j + 1:16 + j + 2],
                           mbt[:, 32 + j + 1:32 + j + 2], ALU.mult, ALU.add,
                           [("biasT", h), "mb"], [("btmp", (j + 1) % 2)])
                    cs = 128 * (j + 1 - 4 * g) - c0
                    tt("pool", pbuf[:, cs:cs + 128], pbuf[:, cs:cs + 128], bt, ALU.mult,
                       [pkey, ("btmp", (j + 1) % 2)], [pkey])
                if knd == "own" and in_group:
                    memset("pool", pbuf[64:128, 0:64], 0.0, [], [pkey])

            def stage2(idx):
                g, ti, nt, kt, c0, knd, j, sub = items[idx]
                slot = idx % 4
                pbuf, pkey = PBUF[slot], ("pT", slot)
                ncol = 512 - c0
                kc = slice(kt * 128, (kt + 1) * 128)
                first, last = ti == 0, ti == nt - 1
                if diff:
                    ob, okey = ps[4 + sub], "ps%d" % (4 + sub)
                    mm(ob[:, c0:512], Vb[:, kc], pbuf[:, 0:ncol], first, last, ["V_h", pkey], [okey])
                    db, dkey = ps[6 + sub], "ps%d" % (6 + sub)
                    mm(db[:, c0:512], ones_b[:], pbuf[:, 0:ncol], first, last, ["ones_b", pkey], [dkey])
                else:
                    ob, okey = ps[2 + g % 2], "ps%d" % (2 + g % 2)
                    mm(ob[:, c0:512], Vb[:, kc], pbuf[:, 0:ncol], first, last, ["V_h", pkey], [okey])
                if last and sub == nsub - 1:
                    finalize(g)

            def finalize(g):
                qs = slice(g * 512, (g + 1) * 512)
                s_a = bcslot[0][:, 0:512]
                s_b = bcslot[0][:, 512:1024]
                s_c = bcslot[1][:, 0:512]
                s_d = bcslot[1][:, 512:1024]
                rhi = BC[:, 2048:2560]
                rlo = BC[:, 2560:3072]
                if diff:
                    cp("dve", s_a, ps[6][:, :], ["ps6"], ["s_a"])
                    cp("dve", s_b, ps[7][:, :], ["ps7"], ["s_b"])
                    cp("dve", s_c, ps[4][:, :], ["ps4"], ["s_c"])
                    cp("dve", s_d, ps[5][:, :], ["ps5"], ["s_d"])
                    recip(s_a, s_a, ["s_a"], ["s_a"])
                    recip(s_b, s_b, ["s_b"], ["s_b"])
                    tt("dve", s_a, s_c, s_a, ALU.mult, ["s_c", "s_a"], ["s_a"])
                    stt(s_b, s_d, misc[:, 3:4], s_b, ALU.mult, ALU.mult, ["s_d", "s_b", "misc3"], ["s_b"])
                    tt("dve", s_a, s_a, s_b, ALU.add, ["s_a", "s_b"], ["s_a"])
                    tt("dve", sqb, s_a, s_a, ALU.mult, ["s_a"], ["sqb"])
                    def fin_b_diff(idx_, s_a=s_a, s_c=s_c, qs=qs, g=g):
                        bn = sbanks[idx_ % 4]
                        mm(ps[bn][:, :], ones_b[:], sqb, True, True, ["sqb", "ones_b"], ["ps%d" % bn])
                        rsqrt_chain(s_c, ps[bn][:, :], 1.0 / 128, ["ps%d" % bn], ["s_c"])
                        stt(attnT[:, 4 + h, qs], s_a, misc[:, 8:9], s_c, ALU.mult, ALU.mult,
                            ["s_a", "s_c", "misc8"], [("at", g)])
                    pending.append([11, fin_b_diff])
                else:
                    ob, okey = ps[2 + g % 2], "ps%d" % (2 + g % 2)
                    r0 = 64 if h % 2 == 0 else 0
                    rhi = BC[:, 2048:2560] if h % 2 == 0 else BC[:, 3072:3584]
                    rlo = BC[:, 2560:3072] if h % 2 == 0 else BC[:, 3584:4096]
                    o0 = 0 if h % 2 == 0 else 64
                    rr, orows = slice(r0, r0 + 1), slice(o0, o0 + 64)
                    recip(s_a[rr, :], ob[rr, :], [okey], ["s_a"])
                    cp("dve", rhi[rr, :], s_a[rr, :], ["s_a"], ["s_c"])
                    tt("dve", s_a[rr, :], s_a[rr, :], rhi[rr, :], ALU.subtract, ["s_a", "s_c"], ["s_a"])
                    cp("dve", rlo[rr, :], s_a[rr, :], ["s_a"], ["s_c"])
                    def fin_b(idx_, rhi=rhi, rlo=rlo, orows=orows, ob=ob, okey=okey, qs=qs, s_b=s_b, g=g):
                        mm(ps[4][:, :], ones_b[:, :], rhi[:, :], True, False, ["s_c", "ones_b"], ["ps4"])
                        mm(ps[4][:, :], ones_b[:, :], rlo[:, :], False, True, ["s_c", "ones_b"], ["ps4"])
                        cp("act", s_b[orows, :], ps[4][orows, :], ["ps4"], ["s_b"])
                        tt("dve", attnT[orows, h // 2, qs], ob[orows, :], s_b[orows, :], ALU.mult, [okey, "s_b"],
                           [("at", g)])
                    pending.append([6, fin_b])

            n = len(items)
            pending = []
            for idx in range(n + LOOK):
                if idx < n:
                    stage1(idx)
                if idx - LOOK >= 0:
                    stage2(idx - LOOK)
                for pe_ in list(pending):
                    pe_[0] -= 1
                    if pe_[0] <= 0:
                        pending.remove(pe_)
                        pe_[1](idx)
            for pe_ in pending:
                pe_[1](n + LOOK - 1)

        HTK = [("hT", b) for b in range(8)]

        fence(["HB", "kT", "qT", "V_h"])
        fence(["mi_tmp", "Rt", "rsf", "sqb"] + [("pT", a_) for a_ in range(4)])
        fence([("bc", 0), ("bc", 1), ("bc", 2), "s_a", "s_b", "s_c", "s_d", "t1", "t2"])
        memset("pool", qT[64:128, :], 0.0, [], ["qT"])
        memset("pool", qTb[0:64, :], 0.0, [], ["qT", ("bc", 2), "t1", "t2"])
        Vd = Vb.rearrange("p (t e) -> p t e", t=32)
        for h in range(4):
            cq0, ck0, cv0 = 672 + h * 128, 1184 + h * 128, 1696 + h * 128
            jobs = [("k", ck0, b_, kT, "kT") for b_ in range(8)] + [("q", cq0, b_, qT, "qT") for b_ in range(4)]

            def b_part1(i):
                side, c0w, blk, dst, dkey = jobs[i]
                pa, pak = ps[PJ_PA[i % 4]], "ps%d" % PJ_PA[i % 4]
                nb, nbk = ps[PJ_NB[i % 2]], "ps%d" % PJ_NB[i % 2]
                for k in range(8):
                    mm(pa[:, :], win[:, k, c0w:c0w + 128], hT[:, blk, k, :], k == 0, k == 7,
                       [("win", k), ("hT", blk)], [pak])

            def b_part1b(i):
                pa, pak = ps[PJ_PA[i % 4]], "ps%d" % PJ_PA[i % 4]
                nb, nbk = ps[PJ_NB[i % 2]], "ps%d" % PJ_NB[i % 2]
                act(sqb, pa[:, :], AF.Square, [pak], ["sqb"])
                mm(nb[:, :], blk64[:], sqb, True, True, ["sqb", "blk64"], [nbk])

            def b_part2(i):
                side, c0w, blk, dst, dkey = jobs[i]
                bs = slice(blk * 512, (blk + 1) * 512)
                pa, pak = ps[PJ_PA[i % 4]], "ps%d" % PJ_PA[i % 4]
                nb, nbk = ps[PJ_NB[i % 2]], "ps%d" % PJ_NB[i % 2]
                rs, rsk = rsfs[i % 2], rsfk[i % 2]
                rsqrt_chain(rs, nb[:, :], 1.0 / 64, [nbk], rsk)
                if side == "k":
                    stt(dst[:, bs], pa[:, :], misc[:, 0:1], rs, ALU.mult, ALU.mult, [pak, "misc0"] + rsk, [dkey])
                else:
                    tt("dve", qT[0:64, bs], pa[0:64, :], rs[0:64, :], ALU.mult, [pak] + rsk, [dkey])
                    tt("dve", qTb[64:128, bs], pa[64:128, :], rs[64:128, :], ALU.mult, [pak] + rsk, [dkey])

            b_part1(0)
            b_part1(1)
            for i in range(len(jobs) + 1):
                if i < len(jobs):
                    b_part1b(i)
                if i + 2 < len(jobs):
                    b_part1(i + 2)
                if i >= 1:
                    b_part2(i - 1)
            for t in range(32):
                blk, tc_ = t // 4, (t % 4) * 128
                vbn = 3 if (t // 4) % 2 == 0 else 6
                for k in range(8):
                    mm(ps[vbn][:, (t % 4) * 128:(t % 4 + 1) * 128], hT[:, blk, k, tc_:tc_ + 128],
                       win[:, k, cv0:cv0 + 128], k == 0, k == 7, [("win", k), ("hT", blk)], ["ps%d" % vbn])
                if t % 4 == 3:
                    cp("act", Vd[:, t - 3:t + 1, :], ps[vbn][:, :].rearrange("p (t e) -> p t e", t=4), ["ps%d" % vbn],
                       ["V_h"])
            attention(h, "diff")

        if stop == 'B':
            return finish()
        U1f = U1
        fence(["qT", ("bc", 2), "t1", "t2"])

        def blkv(blk, off, n):
            return U1f[:, blk * 4096 + off:blk * 4096 + off + n]

        import os as _os
        for blk in range(int(_os.environ.get('KBLK', '8'))):
            own = blk < 4
            banks = {}
            for m in range(2):
                for k in range(8):
                    mm(ps[m][:, :], win[:, k, 384 + m * 128:384 + (m + 1) * 128], hT[:, blk, k, :], k == 0, k == 7,
                       [("win", k), ("hT", blk)], ["ps%d" % m])
            if own:
                for m in range(3):
                    for k in range(8):
                        mm(ps[2 + m][:, :], win[:, k, m * 128:(m + 1) * 128], hT[:, blk, k, :], k == 0, k == 7,
                           [("win", k), ("hT", blk)], ["ps%d" % (2 + m)])
            for k in range(8):
                mm(ps[5][0:96, :], kpeW3[:, k, :], hT[:, blk, k, :], k == 0, k == 7, ["kpeW", ("hT", blk)], ["ps5"])
            for k in range(8):
                mm(ps[6][0:96, :], kpeWr3[:, k, :], hT[:, blk, k, :], k == 0, k == 7, ["kpeWr", ("hT", blk)], ["ps6"])
            if stop == 'Ca':
                return finish()
            hk = ("hT", blk)
            for m in range(2):
                act(sqb, ps[m][:, :], AF.Square, ["ps%d" % m], ["sqb"])
                mm(ps[7][:, :], ones_b[:], sqb, m == 0, m == 1, ["sqb", "ones_b"], ["ps7"])
            rsqrt_chain(rsf, ps[7][:, :], 1.0 / 256, ["ps7"], ["rsf"])
            for m in range(2):
                stt(blkv(blk, m * 512, 512), ps[m][:, :], col[:, C_CKVG + m:C_CKVG + m + 1], rsf, ALU.mult, ALU.mult,
                    ["ps%d" % m, "rsf", "col"], [hk])
            if stop == 'Cb':
                return finish()
            if own:
                for m in range(3):
                    act(sqb, ps[2 + m][:, :], AF.Square, ["ps%d" % (2 + m)], ["sqb"])
                    mm(ps[7][:, :], ones_b[:], sqb, m == 0, m == 2, ["sqb", "ones_b"], ["ps7"])
                rsqrt_chain(rsf, ps[7][:, :], 1.0 / 384, ["ps7"], ["rsf"])
                for m in range(3):
                    stt(blkv(blk, 1024 + m * 512, 512), ps[2 + m][:, :], col[:, C_CQG + m:C_CQG + m + 1], rsf,
                        ALU.mult, ALU.mult, ["ps%d" % (2 + m), "rsf", "col"], [hk])
            if stop == 'Cc':
                return finish()
            _sk = _os.environ.get('KSKIP', '')
            if 'a' not in _sk:
                act(blkv(blk, 3072, 512)[0:96, :], ps[5][0:96, :], AF.Square, ["ps5"], [hk])
                memset("pool", blkv(blk, 3072, 512)[96:128, :], 0.0, [], [hk])
            bsl = slice(blk * 512, (blk + 1) * 512)
            t1 = bcslot[2][:, 0:512]
            t2 = bcslot[2][:, 512:1024]
            if 'b' not in _sk:
                stt(t1[:, :], ps[5][:, :], col[:, C_KG:C_KG + 1], C1[:, bsl], ALU.mult, ALU.mult,
                    ["ps5", "col", "C1"], ["t1"])
            if 'c' not in _sk:
                stt(t2[:, :], ps[6][:, :], col[:, C_KGR:C_KGR + 1], C2[:, bsl], ALU.mult, ALU.mult,
                    ["ps6", "col", "C2"], ["t2"])
            if 'd' not in _sk:
                tt("dve", blkv(blk, 2560, 512)[:, :], t1[:, :], t2[:, :], ALU.add, ["t1", "t2"], [hk])

        if stop == 'Cd' :
            return finish()
        if stop == 'C1':
            return finish()
        wuq = U2[:, 0:2304].rearrange("p (k n) -> p k n", k=3)
        wuqr = U2[:, 2304:4608].rearrange("p (k n) -> p k n", k=3)
        wukv = U2[:, 4608:6656].rearrange("p (k n) -> p k n", k=2)
        w_uq_v = w_uq.rearrange("(k p) n -> p k n", p=128)
        w_uq_v4 = w_uq.rearrange("(k p) (h d) -> p k h d", p=128, d=96)
        wuqr4 = U2[:, 2304:4608].rearrange("p (k h d) -> p k h d", k=3, d=96)
        dma("pool", wuq, w_uq_v, "wuq", [], WIN)
        memset("dve", U2[:, 2304:4608], 0.0, [], WIN)
        for k in range(3):
            dma("pool", wuqr4[:, k, :, 64:80], w_uq_v4[:, k, :, 80:96], "wuqr", [], WIN)
            dma("pool", wuqr4[:, k, :, 80:96], w_uq_v4[:, k, :, 64:80], "wuqr", [], WIN)
        dma("pool", wukv, w_ukv.rearrange("(k p) n -> p k n", p=128), "wukv", [], WIN)
        wo = U2[:, 8192:8192 + 8192].rearrange("p (k n) -> p k n", k=8)
        dma("pool", wo, w_o.rearrange("(k p) n -> p k n", p=128), "wo", [], WIN)

        if stop == 'C':
            return finish()
        Vm = Vb.rearrange("p (t e) -> p t e", t=32)
        memset("pool", kT[96:128, :], 0.0, [], ["kT"])
        sqk = MI[:, 2560:3072]
        memset("pool", sqk, 0.0, [], [("btmp", 0), ("btmp", 1), "sqk"])
        memset("pool", sqb[96:128, :], 0.0, [], ["sqb"])
        memset("pool", BC[:, 2048:4096], 0.0, [], ["s_c", "s_d", ("bc", 1)])
        for h in range(8):
            even = h % 2 == 0
            memset("pool", Vb, 0.0, [], ["V_h"])
            memset("pool", Vm[:, :, 64:65] if even else Vm[:, :, 0:1], 1.0, [], ["V_h"])
            def k_part1(blk):
                hk = ("hT", blk)
                pa, pak = ps[PJ_PA[blk % 4]], "ps%d" % PJ_PA[blk % 4]
                nb, nbk = ps[PJ_NB[blk % 2]], "ps%d" % PJ_NB[blk % 2]
                for m in range(2):
                    mm(pa[:, :], wukv[:, m, h * 128:(h + 1) * 128], blkv(blk, m * 512, 512), m == 0, m == 1,
                       WIN + [hk], [pak])

            def k_part1b(blk):
                hk = ("hT", blk)
                pa, pak = ps[PJ_PA[blk % 4]], "ps%d" % PJ_PA[blk % 4]
                nb, nbk = ps[PJ_NB[blk % 2]], "ps%d" % PJ_NB[blk % 2]
                act(sqk[0:64, :], pa[0:64, :], AF.Square, [pak], ["sqk"])
                mm(nb[:, :], ones_b[:, :], sqk[:, :], True, False, ["sqk", "ones_b"], [nbk])
                mm(nb[:, :], ones_b[:, :], blkv(blk, 3072, 512)[:, :], False, True, [hk, "ones_b"], [nbk])

            def k_part2(blk):
                hk = ("hT", blk)
                bs = slice(blk * 512, (blk + 1) * 512)
                pa, pak = ps[PJ_PA[blk % 4]], "ps%d" % PJ_PA[blk % 4]
                nb, nbk = ps[PJ_NB[blk % 2]], "ps%d" % PJ_NB[blk % 2]
                rs, rsk = rsfs[blk % 2], rsfk[blk % 2]
                rsqrt_chain(rs[0:96, :], nb[0:96, :], 1.0 / 96, [nbk], rsk, pr=(0, 96))
                stt(kT[0:64, bs], pa[0:64, :], col[0:64, C_KG:C_KG + 1], rs[0:64, :], ALU.mult, ALU.mult,
                    [pak, "col"] + rsk, ["kT"])
                tt("dve", kT[64:96, bs], blkv(blk, 2560, 512)[64:96, :], rs[64:96, :], ALU.mult, [hk] + rsk, ["kT"])

            k_part1(0)
            k_part1(1)
            for i in range(9):
                if i < 8:
                    k_part1b(i)
                if i + 2 < 8:
                    k_part1(i + 2)
                if i >= 1:
                    k_part2(i - 1)
            for t in range(32):
                blk, tc_ = t // 4, (t % 4) * 128
                vbn = 3 if (t // 4) % 2 == 0 else 6
                for m in range(2):
                    mm(ps[vbn][:, (t % 4) * 64:(t % 4 + 1) * 64], blkv(blk, m * 512, 512)[:, tc_:tc_ + 128],
                       wukv[:, m, h * 128 + 64:h * 128 + 128], m == 0, m == 1, WIN + [("hT", blk)], ["ps%d" % vbn])
                if t % 4 == 3:
                    dstv = Vm[:, t - 3:t + 1, 0:64] if even else Vm[:, t - 3:t + 1, 64:128]
                    cp("act", dstv, ps[vbn][:, 0:256].rearrange("p (t e) -> p t e", t=4), ["ps%d" % vbn], ["V_h"])
            QB = [(0, 1), (4, 5)]

            def q_part1(blk):
                hk = ("hT", blk)
                nb, nbk = ps[PJ_NB[blk % 2]], "ps%d" % PJ_NB[blk % 2]
                for (bank, wbase) in ((QB[blk % 2][0], 0), (QB[blk % 2][1], 2304)):
                    for m in range(3):
                        w0 = wbase + m * 768 + h * 96
                        mm(ps[bank][:, :], U2[:, w0:w0 + 128], blkv(blk, 1024 + m * 512, 512),
                           m == 0, m == 2, WIN + [hk], ["ps%d" % bank])
                ba = QB[blk % 2][0]
                act(sqb[0:96, :], ps[ba][0:96, :], AF.Square, ["ps%d" % ba], ["sqb"])
                mm(nb[:, :], ones_b[:, :], sqb[:, :], True, True, ["sqb", "ones_b"], [nbk])

            def q_part2(blk):
                bs = slice(blk * 512, (blk + 1) * 512)
                nb, nbk = ps[PJ_NB[blk % 2]], "ps%d" % PJ_NB[blk % 2]
                ba, bb = QB[blk % 2]
                rs, rsk = rsfs[blk % 2], rsfk[blk % 2]
                rsqrt_chain(rs[0:96, :], nb[0:96, :], 1.0 / 96, [nbk], rsk, pr=(0, 96))
                t1 = bcslot[2][:, 0:512]
                t2 = bcslot[2][:, 512:1024]
                stt(t1[0:96, :], ps[ba][0:96, :], col[0:96, C_QG:C_QG + 1], C1[0:96, bs], ALU.mult, ALU.mult,
                    ["ps%d" % ba, "col", "C1"], ["t1"])
                stt(t2[0:96, :], ps[bb][0:96, :], col[0:96, C_QGR:C_QGR + 1], C2[0:96, bs], ALU.mult, ALU.mult,
                    ["ps%d" % bb, "col", "C2"], ["t2"])
                tt("dve", t1[0:96, :], t1[0:96, :], t2[0:96, :], ALU.add, ["t1", "t2"], ["t1"])
                tt("dve", qT[0:96, bs], t1[0:96, :], rs[0:96, :], ALU.mult, ["t1"] + rsk, ["qT"])

            for i in range(5):
                if i < 4:
                    q_part1(i)
                if i >= 1:
                    q_part2(i - 1)
            attention(h, "mla")

        if stop == 'D':
            return finish()
        ATK = [("at", g) for g in range(4)]
        fence([("bc", 0), ("bc", 1), ("bc", 2), "s_a", "s_b", "s_c", "s_d", "t1", "t2"])
        fence(["C1", "C2", "xs2", ("h2f", 0), ("h2f", 1), "h2Tf"])
        expand(modcol[:, 16:24], 0, ["modcol"], 6)
        expand(gcol[:, 8:16], 1, ["gcol"], 6)
        expand(modcol[:, 24:32], 2, ["modcol"], 6)
        x1 = U1.bitcast(F32).rearrange("p (s n) -> p s n", s=16)
        h2T = attnT
        xs2 = RA[:, 0:2048].bitcast(F32)
        h2f = RA[:, 2048:4096].bitcast(F32)
        h2Tf = RA[:, 4096:6144].bitcast(F32).rearrange("p (k c) -> p k c", k=8)
        gate3 = gate[:].rearrange("p (s e) -> p s e", s=16)
        wr3 = wr_f[:].rearrange("p (k n) -> p k n", k=8)
        h2fs = [RA[:, 2048:4096].bitcast(F32), RA[:, 6144:8192].bitcast(F32)]

        def e_part1(s):
            h2f, h2fk = h2fs[s % 2], ("h2f", s % 2)
            sc = slice(s * 128, (s + 1) * 128)
            hk = ("hT", s // 2)
            dma("sp", xs2, xp[s * 128:(s + 1) * 128, :], "xs2", [], ["xs2"])
            wob = (0, 1) if s % 2 == 0 else (5, 6)
            for half in range(2):
                for c in range(8):
                    mm(ps[wob[half]][:, :], attnT[:, c, sc], wo[:, c, half * 512:(half + 1) * 512], c == 0, c == 7,
                       ATK + WIN, ["ps%d" % wob[half]])
            for half in range(2):
                hs = slice(half * 512, (half + 1) * 512)
                tt("dve", x1[:, s, hs], ps[wob[half]][:, :], bcslot[0][:, hs], ALU.mult,
                   ["ps%d" % wob[half], ("bc", 0)], [hk, ("x1", s)])
                tt("dve", x1[:, s, hs], x1[:, s, hs], xs2[:, hs], ALU.add, ["xs2", ("x1", s)], [("x1", s)])
            act(h2f, x1[:, s, :], AF.Square, [("x1", s)], [h2fk, "stat2"], accum_out=stat2[:, s:s + 1])
            rsqrt_chain(stat2[:, 32 + s:33 + s], stat2[:, s:s + 1], 1.0 / 1024, ["stat2"], ["stat2"])
            stt(h2f, x1[:, s, :], stat2[:, 32 + s:33 + s], bcslot[1], ALU.mult, ALU.mult,
                [("x1", s), "stat2", ("bc", 1)], [h2fk])
            tt("dve", h2f, h2f, bcslot[2], ALU.add, [h2fk, ("bc", 2)], [h2fk])

        def e_part2(s):
            h2f, h2fk = h2fs[s % 2], ("h2f", s % 2)
            sc = slice(s * 128, (s + 1) * 128)
            for k in range(8):
                tr(ps[2 + k // 4][:, (k % 4) * 128:(k % 4 + 1) * 128], h2f[:, k * 128:(k + 1) * 128], ident_f[:],
                   [h2fk, "ident_f"], ["ps%d" % (2 + k // 4)])
            for hf in range(2):
                cp("act", h2T[:, hf * 4:(hf + 1) * 4, sc], ps[2 + hf][:, :].rearrange("p (k c) -> p k c", k=4),
                   ["ps%d" % (2 + hf)], ATK + [("h2T", s)])
                cp("dve", h2Tf[:, hf * 4:(hf + 1) * 4, :], ps[2 + hf][:, :].rearrange("p (k c) -> p k c", k=4),
                   ["ps%d" % (2 + hf)], ["h2Tf"])
            for k in range(8):
                mm(ps[4][:, 0:20], h2Tf[:, k, :], wr3[:, k, :], k == 0, False, ["h2Tf", "wr"], ["ps4"])
            mm(ps[4][:, 0:20], ones_f[0:1, :], br_f[0:1, :], False, True, ["ones_f", "br"], ["ps4"])
            R = rt
            cp("dve", R[:, 0:20], ps[4][:, 0:20], ["ps4"], ["rt"])
            RK = ["rt"]
            rmax(R[:, 20:21], R[:, 0:4], RK, RK)
            ts("dve", R[:, 24:28], R[:, 0:4], R[:, 20:21], None, ALU.is_equal, None, RK, RK)
            ts("dve", R[:, 21:22], R[:, 20:21], -1.0, None, ALU.mult, None, RK, RK)
            act(R[:, 28:32], R[:, 0:4], AF.Exp, RK, RK, bias=R[:, 21:22], accum_out=R[:, 22:23])
            recip(R[:, 23:24], R[:, 22:23], RK, RK)
            ts("dve", R[:, 32:36], R[:, 4:8], R[:, 24:25], None, ALU.mult, None, RK, RK)
            for g_ in range(1, 4):
                stt(R[:, 32:36], R[:, 4 + 4 * g_:8 + 4 * g_], R[:, 24 + g_:25 + g_], R[:, 32:36], ALU.mult, ALU.add,
                    RK, RK)
            rmax(R[:, 36:37], R[:, 32:36], RK, RK)
            ts("dve", R[:, 40:44], R[:, 32:36], R[:, 36:37], None, ALU.is_equal, None, RK, RK)
            stt(R[:, 44:48], R[:, 40:44], -1e30, R[:, 32:36], ALU.mult, ALU.add, RK, RK)
            rmax(R[:, 37:38], R[:, 44:48], RK, RK)
            ts("dve", R[:, 48:52], R[:, 44:48], R[:, 37:38], None, ALU.is_equal, None, RK, RK)
            tt("dve", R[:, 38:39], R[:, 37:38], R[:, 36:37], ALU.subtract, RK, RK)
            act(R[:, 39:40], R[:, 38:39], AF.Exp, RK, RK)
            ts("dve", R[:, 52:53], R[:, 39:40], 1.0, None, ALU.add, None, RK, RK)
            recip(R[:, 53:54], R[:, 52:53], RK, RK)
            tt("dve", R[:, 53:54], R[:, 53:54], R[:, 23:24], ALU.mult, RK, RK)
            tt("dve", R[:, 54:55], R[:, 53:54], R[:, 39:40], ALU.mult, RK, RK)
            ts("dve", R[:, 56:60], R[:, 40:44], R[:, 53:54], None, ALU.mult, None, RK, RK)
            stt(R[:, 56:60], R[:, 48:52], R[:, 54:55], R[:, 56:60], ALU.mult, ALU.add, RK, RK)
            for g_ in range(4):
                ts("dve", gate3[:, s, 4 * g_:4 * g_ + 4], R[:, 56:60], R[:, 24 + g_:25 + g_], None, ALU.mult, None,
                   RK, ["gate"])


        e_part1(0)
        for s in range(16):
            if s + 1 < 16:
                e_part1(s + 1)
            e_part2(s)
        if stop == 'E':
            return finish()
        expand(modcol[:, 40:48], 0, ["modcol"], 6)
        fence(WIN + [(("wb", i_), t_) for i_ in range(2) for t_ in "gud"] + [("wb", 0), ("wb", 1)])
        fence(["kT", "qT", "V_h", "HB", "wds"])
        fence(["mi_tmp", "Rt", "rsf", "sqb", "sqk", ("btmp", 0), ("btmp", 1), "sgt"] + [("pT", a_) for a_ in range(4)]
              + [("biasT", h_) for h_ in range(4)] + [("heT", j_) for j_ in range(4)])
        wbuf = [U2[:, i * 12288:(i + 1) * 12288] for i in range(2)]
        wd_stage = HB[:, 0:8192].bitcast(F32).rearrange("p (j n) -> p j n", j=4)
        heT = [MI[:, i * 512:(i + 1) * 512] for i in range(4)]
        sgt = MI[:, 2048:3072].bitcast(F32)
        H2K = [("h2T", s) for s in range(16)]
        for e in range(N_EXP_RUN):
            wb = wbuf[e % 2]
            wg = wb[:, 0:4096].rearrange("p (k n) -> p k n", k=8)
            wu = wb[:, 4096:8192].rearrange("p (k n) -> p k n", k=8)
            wd = wb[:, 8192:12288].rearrange("p (j n) -> p j n", j=4)
            wk = ("wb", e % 2)
            dma("pool", wg, w_gate[e].rearrange("(k p) n -> p k n", p=128), "wg%d" % (e % 2), [], [wk, (wk, "g")])
            dma("pool", wu, w_up[e].rearrange("(k p) n -> p k n", p=128), "wu%d" % (e % 2), [], [(wk, "u")])
            dma("sp", wd_stage, w_down[e].rearrange("(j p) n -> p j n", p=128), "wds", [], ["wds"])
            for j in range(4):
                tt("pool", wd[:, j, :], wd_stage[:, j, :], bcslot[0], ALU.mult, ["wds", ("bc", 0)], [(wk, "d")])
            for blk in range(4):
                bs = slice(blk * 512, (blk + 1) * 512)
                for j in range(4):
                    for k in range(8):
                        mm(ps[0 + j % 2][:, :], wg[:, k, j * 128:(j + 1) * 128], h2T[:, k, bs], k == 0, k == 7,
                           H2K + [wk, (wk, "g")], ["ps%d" % (j % 2)])
                    for k in range(8):
                        mm(ps[2 + j % 2][:, :], wu[:, k, j * 128:(j + 1) * 128], h2T[:, k, bs], k == 0, k == 7,
                           H2K + [(wk, "u")], ["ps%d" % (2 + j % 2)])
                    act(sgt, ps[j % 2][:, :], AF.Silu, ["ps%d" % (j % 2)], ["sgt"])
                    tt("dve", heT[j], sgt, ps[2 + j % 2][:, :], ALU.mult, ["sgt", "ps%d" % (2 + j % 2)], [("heT", j)])
                for tt_ in range(4):
                    s = blk * 4 + tt_
                    for half in range(2):
                        bank = 4 + (tt_ * 2 + half) % 4
                        for j in range(4):
                            mm(ps[bank][:, :], heT[j][:, tt_ * 128:(tt_ + 1) * 128], wd[:, j, half * 512:(half + 1) * 512],
                               j == 0, j == 3, [("heT", j), (wk, "d")], ["ps%d" % bank])
                        hs = slice(half * 512, (half + 1) * 512)
                        stt(x1[:, s, hs], ps[bank][:, :], gate3[:, s, e:e + 1], x1[:, s, hs], ALU.mult, ALU.add,
                            ["ps%d" % bank, "gate", ("x1", s)], [("x1", s)])
        return finish()


def _own_tiles(half):
    return [j for j in range(32) if ((j % 4) in (0, 3)) == (half == 0)]


_NC_CACHE = {}


def kernel(**inputs):
    x = np.ascontiguousarray(inputs["x"], dtype=np.float32)
    pos = np.asarray(inputs["positions"]).astype(np.int32)
    f = lambda k: np.ascontiguousarray(np.asarray(inputs[k], dtype=np.float32)[0])
    inv_freq = (10000.0 ** (-np.arange(0, 16, dtype=np.float32) / 16.0)).astype(np.float32)
    cst = np.zeros((128, 8), np.float32)
    cst[:, 1] = 0.5 * math.pi
    cst[:, 2] = 0.0
    for i in range(16):
        cst[64 + i, 0] = inv_freq[i]
        cst[80 + i, 0] = inv_freq[i]
        cst[64 + i, 2] = math.pi
    w_r = np.ascontiguousarray(np.concatenate([f("w_rg"), f("w_re")], axis=1))
    b_r = np.ascontiguousarray(np.concatenate([f("b_rg"), f("b_re")])[None, :])
    in_maps = []
    metas = []
    for c in range(8):
        b, half = c // 2, c % 2
        own = _own_tiles(half)
        oth = [j for j in range(32) if j not in own]
        order = own + oth
        xt = x[b].reshape(32, 128, 1024)[order].reshape(4096, 1024)
        pt = pos[b].reshape(32, 128)[order].reshape(1, 4096)
        V = np.zeros((96, 128), np.float32)
        V[0:8] = np.asarray(inputs["c"], np.float32)[b].reshape(8, 128)
        V[8:56] = f("b_ada").reshape(48, 128)
        V[56:64] = f("norm1_g").reshape(8, 128)
        V[64:72] = f("norm2_g").reshape(8, 128)
        V[72:75] = f("mla_cq_g").reshape(3, 128)
        V[75:77] = f("mla_ckv_g").reshape(2, 128)
        V[77, 0:96] = f("mla_q_g")
        V[78, 0:96] = f("mla_k_g")
        V[79, 0:64] = f("diff_q_g"); V[79, 64:128] = f("diff_q_g")
        V[80, 0:64] = f("diff_k_g"); V[80, 64:128] = f("diff_k_g")
        V[81, :] = f("diff_subln_g")
        V[82, 0:64] = f("lambda_q1"); V[83, 0:64] = f("lambda_k1")
        V[84, 0:64] = f("lambda_q2"); V[85, 0:64] = f("lambda_k2")
        qg, kg = f("mla_q_g"), f("mla_k_g")
        V[86, 64:80] = qg[80:96]; V[86, 80:96] = qg[64:80]
        V[87, 64:80] = kg[80:96]; V[87, 80:96] = kg[64:80]
        mbv = np.zeros((128, 48), np.float32)
        for i in range(16):
            vis = oth[i] < own[i]
            mbv[:, i] = 0.0 if vis else NEG
            mbv[:, 16 + i] = 0.0 if vis else 1.0
            mbv[:, 32 + i] = 1.0 if vis else 0.0
        in_maps.append({
            "xp": np.ascontiguousarray(xt), "posp": np.ascontiguousarray(pt), "vrows": V, "cst": cst, "mb": mbv,
            "rel_bias": np.ascontiguousarray(np.asarray(inputs["rel_bias"], np.float32).reshape(1, 128)),
            "w_ada": f("w_ada"), "w_in": f("w_in"), "w_uq": f("w_uq"), "w_ukv": f("w_ukv"), "w_o": f("w_o"),
            "w_r": w_r, "b_r": b_r, "w_gate": f("w_gate"), "w_up": f("w_up"), "w_down": f("w_down"),
        })
        metas.append((b, own))
    if "nc" not in _NC_CACHE:
        _NC_CACHE["nc"] = build_program()
    res = run_bass_kernel_spmd(_NC_CACHE["nc"], in_maps, core_ids=list(range(8)))
    outp = np.zeros((4, 32, 128, 1024), np.float32)
    for c in range(8):
        b, own = metas[c]
        o = np.asarray(res.results[c]["out"]).reshape(16, 128, 1024)
        outp[b, own] = o
    return outp.reshape(4, 4096, 1024)
```
